# Optimizing a Trainium2 kernel written in Bass

```python
import math
import jax, jax.numpy as jnp
from jax import lax
import numpy as np

D_MODEL = 1024
BATCH = 4
SEQ = 8192
DEPTH = 2

GRID_W = 64
CTX_LEN = 256
N_MIXERS = 2
ATT_HEADS = 8
ATT_HEAD_DIM = D_MODEL // ATT_HEADS // 2
ATT_V_DIM = 2 * ATT_HEAD_DIM
ROPE_THETA = 10000.0
Q_BLOCK = 128
SGU_CHUNK = 128
SGU_DFF = 2 * D_MODEL
SGU_GROUPS = 8
N_EXPERTS = 256
TOP_K = 8
N_GROUPS = 8
TOPK_GROUPS = 4
EXPERT_DFF = D_MODEL // 4
SHARED_DFF = EXPERT_DFF
ROUTED_SCALE = 2.5
MOE_BLOCK = 128
DEEPNORM_ALPHA = (2 * DEPTH) ** 0.25
DEEPNORM_BETA = (8 * DEPTH) ** -0.25
LN_EPS = 1e-5
N_ATTN_LAYERS = (DEPTH + 1) // 2
N_SGU_LAYERS = DEPTH // 2

kernel_name = "hybrid_diffattn_sgu_moe_deepnorm_prefix"


def _layer_norm(x, g, b):
    xf = x.astype(jnp.float32)
    mu = jnp.mean(xf, axis=-1, keepdims=True)
    var = jnp.mean(jnp.square(xf - mu), axis=-1, keepdims=True)
    return ((xf - mu) * lax.rsqrt(var + LN_EPS) * g.astype(jnp.float32) + b.astype(jnp.float32)).astype(x.dtype)


def _rms_norm(x, g):
    xf = x.astype(jnp.float32)
    return (xf * lax.rsqrt(jnp.mean(jnp.square(xf), axis=-1, keepdims=True) + LN_EPS) * g.astype(jnp.float32)).astype(x.dtype)


def _rope_axis(xh, pos):
    half = xh.shape[-1]
    inv = ROPE_THETA ** (-jnp.arange(0, half, 2, dtype=jnp.float32) / half)
    ang = pos.astype(jnp.float32)[:, None] * inv[None, :]
    ang = jnp.concatenate([ang, ang], axis=-1)[None, :, None, None, :]
    x1, x2 = jnp.split(xh.astype(jnp.float32), 2, axis=-1)
    rot = jnp.concatenate([-x2, x1], axis=-1)
    return (xh.astype(jnp.float32) * jnp.cos(ang) + rot * jnp.sin(ang)).astype(xh.dtype)


def _rope_2d(x, row_pos, col_pos):
    xr, xc = jnp.split(x, 2, axis=-1)
    return jnp.concatenate([_rope_axis(xr, row_pos), _rope_axis(xc, col_pos)], axis=-1)


def _diff_attn_core(q, k, v, lam):
    s = jnp.einsum('bqhmd,bkhmd->bhmqk', q, k, preferred_element_type=jnp.float32) * (ATT_HEAD_DIM ** -0.5)
    p = jax.nn.softmax(s, axis=-1)
    pd = p[:, :, 0] - lam * p[:, :, 1]
    o = jnp.einsum('bhqk,bkhe->bqhe', pd, v.astype(jnp.float32))
    return o.astype(v.dtype)


def _diff_attn_out(o, subln_g, lam_init, w_out):
    o = _rms_norm(o, subln_g) * (1.0 - lam_init)
    return o.reshape(o.shape[0], o.shape[1], D_MODEL) @ w_out


def _diff_attention(h, hc, w_in, w_out, lam_p, subln_g, lam_init, row_pos, col_pos, ctx_out):
    B, N, _ = h.shape
    C = hc.shape[1]
    lam_p = lam_p.astype(jnp.float32)
    lam = jnp.exp(jnp.sum(lam_p[0] * lam_p[1])) - jnp.exp(jnp.sum(lam_p[2] * lam_p[3])) + lam_init
    qkv = h @ w_in
    q = _rope_2d(qkv[..., :D_MODEL].reshape(B, N, ATT_HEADS, 2, ATT_HEAD_DIM), row_pos, col_pos)
    k = _rope_2d(qkv[..., D_MODEL:2 * D_MODEL].reshape(B, N, ATT_HEADS, 2, ATT_HEAD_DIM), row_pos, col_pos)
    v = qkv[..., 2 * D_MODEL:].reshape(B, N, ATT_HEADS, ATT_V_DIM)
    kvc = hc @ w_in[:, D_MODEL:]
    kc = kvc[..., :D_MODEL].reshape(B, C, ATT_HEADS, 2, ATT_HEAD_DIM)
    vc = kvc[..., D_MODEL:].reshape(B, C, ATT_HEADS, ATT_V_DIM)
    k_all = jnp.concatenate([k, kc], axis=1)
    v_all = jnp.concatenate([v, vc], axis=1)
    qb = q.reshape(B, N // Q_BLOCK, Q_BLOCK, ATT_HEADS, 2, ATT_HEAD_DIM).transpose(1, 0, 2, 3, 4, 5)
    ob = lax.map(lambda qi: _diff_attn_core(qi, k_all, v_all, lam), qb)
    o = ob.transpose(1, 0, 2, 3, 4).reshape(B, N, ATT_HEADS, ATT_V_DIM)
    y = _diff_attn_out(o, subln_g, lam_init, w_out)
    yc = None
    if ctx_out:
        qc = (hc @ w_in[:, :D_MODEL]).reshape(B, C, ATT_HEADS, 2, ATT_HEAD_DIM)
        yc = _diff_attn_out(_diff_attn_core(qc, kc, vc, lam), subln_g, lam_init, w_out)
    return y, yc


def _sgu_mixer(h, w_in, b_in, norm_g, norm_b, w_s, b_s, w_out):
    B, N, _ = h.shape
    z = jax.nn.gelu(h @ w_in + b_in, approximate=False)
    u, v = jnp.split(z, 2, axis=-1)
    v = _layer_norm(v, norm_g, norm_b)
    vc = v.reshape(B, N // SGU_CHUNK, SGU_CHUNK, SGU_GROUPS, SGU_DFF // SGU_GROUPS)
    vm = jnp.einsum('gpq,bnqgc->bnpgc', w_s, vc) + b_s.T[:, :, None]
    return (u * vm.reshape(B, N, SGU_DFF)) @ w_out


def _route(xt, w_r, r_bias):
    T = xt.shape[0]
    scores = jax.nn.sigmoid(xt.astype(jnp.float32) @ w_r.astype(jnp.float32))
    choice = scores + r_bias.astype(jnp.float32)
    grp = choice.reshape(T, N_GROUPS, N_EXPERTS // N_GROUPS)
    gscore = jnp.sum(lax.top_k(grp, 2)[0], axis=-1)
    _, gidx = lax.top_k(gscore, TOPK_GROUPS)
    gmask = jnp.any(jax.nn.one_hot(gidx, N_GROUPS, dtype=jnp.bool_), axis=1)
    emask = jnp.repeat(gmask, N_EXPERTS // N_GROUPS, axis=1)
    _, idx = lax.top_k(jnp.where(emask, choice, -jnp.inf), TOP_K)
    w = jnp.take_along_axis(scores, idx, axis=1)
    w = w / jnp.sum(w, axis=-1, keepdims=True) * ROUTED_SCALE
    return idx.astype(jnp.int32), w


def _moe(xt, w_r, r_bias, wg, wu, wd, sg, su, sd):
    T = xt.shape[0]
    idx, w = _route(xt, w_r, r_bias)
    TK = T * TOP_K
    e = idx.reshape(-1)
    tok = jnp.arange(TK, dtype=jnp.int32) // TOP_K
    wf = w.reshape(-1)
    order = jnp.argsort(e)
    se = e[order]
    counts = jnp.bincount(e, length=N_EXPERTS).astype(jnp.int32)
    starts = jnp.cumsum(counts) - counts
    padded = (counts + MOE_BLOCK - 1) // MOE_BLOCK * MOE_BLOCK
    pends = jnp.cumsum(padded)
    pstarts = pends - padded
    dest = pstarts[se] + jnp.arange(TK, dtype=jnp.int32) - starts[se]
    n_blocks = -(-TK // MOE_BLOCK) + N_EXPERTS
    P = n_blocks * MOE_BLOCK
    slot_tok = jnp.zeros((P,), jnp.int32).at[dest].set(tok[order])
    slot_w = jnp.zeros((P,), jnp.float32).at[dest].set(wf[order])
    block_e = jnp.clip(jnp.searchsorted(pends, jnp.arange(n_blocks, dtype=jnp.int32) * MOE_BLOCK, side='right'), 0, N_EXPERTS - 1)

    def step(acc, blk):
        bt, bw, be = blk
        xb = xt[bt]
        hb = jax.nn.silu(xb @ wg[be]) * (xb @ wu[be])
        yb = (hb @ wd[be]) * bw[:, None]
        return acc.at[bt].add(yb.astype(acc.dtype)), None

    routed, _ = lax.scan(step, jnp.zeros_like(xt),
                         (slot_tok.reshape(n_blocks, MOE_BLOCK), slot_w.reshape(n_blocks, MOE_BLOCK), block_e))
    shared = (jax.nn.silu(xt @ sg) * (xt @ su)) @ sd
    return routed + shared


def setup_inputs(seed: int = 0) -> dict:
    key = jax.random.key(seed)
    ks = jax.random.split(key, 32)
    f32 = jnp.float32
    D, dh, dv = D_MODEL, ATT_HEAD_DIM, ATT_V_DIM
    nrm = lambda k, shape, s: jax.random.normal(k, shape, f32) * s
    beta = DEEPNORM_BETA
    return {
        'x': nrm(ks[0], (BATCH, SEQ, D), 1.0),
        'c': nrm(ks[1], (BATCH, D), 1.0),
        'ctx': nrm(ks[2], (BATCH, CTX_LEN, D), 1.0),
        'c_ctx': nrm(ks[3], (D,), 0.5),
        'w_mod': nrm(ks[4], (DEPTH, D, 6 * D), 0.5 * D ** -0.5),
        'b_mod': nrm(ks[5], (DEPTH, 6 * D), 0.02),
        'ln_g': 1.0 + nrm(ks[6], (DEPTH, 2, D), 0.02),
        'ln_b': nrm(ks[7], (DEPTH, 2, D), 0.02),
        'attn_w_in': nrm(ks[8], (N_ATTN_LAYERS, D, 3 * D), D ** -0.5),
        'attn_w_out': nrm(ks[9], (N_ATTN_LAYERS, D, D), beta * D ** -0.5),
        'attn_lambda': nrm(ks[10], (N_ATTN_LAYERS, 4, dh), 0.1),
        'attn_subln_g': 1.0 + nrm(ks[11], (N_ATTN_LAYERS, dv), 0.02),
        'sgu_w_in': nrm(ks[12], (N_SGU_LAYERS, D, 2 * SGU_DFF), D ** -0.5),
        'sgu_b_in': nrm(ks[13], (N_SGU_LAYERS, 2 * SGU_DFF), 0.02),
        'sgu_norm_g': 1.0 + nrm(ks[14], (N_SGU_LAYERS, SGU_DFF), 0.02),
        'sgu_norm_b': nrm(ks[15], (N_SGU_LAYERS, SGU_DFF), 0.02),
        'sgu_w_s': nrm(ks[16], (N_SGU_LAYERS, SGU_GROUPS, SGU_CHUNK, SGU_CHUNK), SGU_CHUNK ** -0.5),
        'sgu_b_s': 1.0 + nrm(ks[17], (N_SGU_LAYERS, SGU_GROUPS, SGU_CHUNK), 0.02),
        'sgu_w_out': nrm(ks[18], (N_SGU_LAYERS, SGU_DFF, D), beta * SGU_DFF ** -0.5),
        'router_w': nrm(ks[19], (DEPTH, D, N_EXPERTS), D ** -0.5),
        'router_bias': nrm(ks[20], (DEPTH, N_EXPERTS), 0.01),
        'exp_w_gate': nrm(ks[21], (DEPTH, N_EXPERTS, D, EXPERT_DFF), D ** -0.5),
        'exp_w_up': nrm(ks[22], (DEPTH, N_EXPERTS, D, EXPERT_DFF), D ** -0.5),
        'exp_w_down': nrm(ks[23], (DEPTH, N_EXPERTS, EXPERT_DFF, D), beta * EXPERT_DFF ** -0.5),
        'sh_w_gate': nrm(ks[24], (DEPTH, D, SHARED_DFF), D ** -0.5),
        'sh_w_up': nrm(ks[25], (DEPTH, D, SHARED_DFF), D ** -0.5),
        'sh_w_down': nrm(ks[26], (DEPTH, SHARED_DFF, D), beta * SHARED_DFF ** -0.5),
    }


def reference(x, c, ctx, c_ctx, w_mod, b_mod, ln_g, ln_b, attn_w_in, attn_w_out, attn_lambda, attn_subln_g,
              sgu_w_in, sgu_b_in, sgu_norm_g, sgu_norm_b, sgu_w_s, sgu_b_s, sgu_w_out,
              router_w, router_bias, exp_w_gate, exp_w_up, exp_w_down, sh_w_gate, sh_w_up, sh_w_down):
    B, N, D = x.shape
    C = ctx.shape[1]
    ROWS = N // GRID_W
    row_pos = jnp.repeat(jnp.arange(ROWS, dtype=jnp.int32), GRID_W, total_repeat_length=ROWS * GRID_W)
    col_pos = jnp.tile(jnp.arange(GRID_W, dtype=jnp.int32), ROWS)
    last_ctx_read = max(i for i in range(DEPTH) if i % N_MIXERS == 0)
    s_c = jax.nn.silu(c)
    s_cc = jax.nn.silu(c_ctx)
    xc = ctx
    for i in range(DEPTH):
        mod = (s_c @ w_mod[i] + b_mod[i])[:, None, :]
        sh_m, sc_m, g_m, sh_f, sc_f, g_f = jnp.split(mod, 6, axis=-1)
        modc = s_cc @ w_mod[i] + b_mod[i]
        shc_m, scc_m, gc_m, shc_f, scc_f, gc_f = jnp.split(modc, 6, axis=-1)
        is_attn = (i % N_MIXERS == 0)
        ctx_update = i < last_ctx_read
        h = x * (1.0 + sc_m) + sh_m
        if is_attn:
            a = i // N_MIXERS
            lam_init = 0.8 - 0.6 * math.exp(-0.3 * i)
            hc = xc * (1.0 + scc_m) + shc_m
            y, yc = _diff_attention(h, hc, attn_w_in[a], attn_w_out[a], attn_lambda[a], attn_subln_g[a],
                                    lam_init, row_pos, col_pos, ctx_update)
        else:
            s = i // N_MIXERS
            sgu_p = (sgu_w_in[s], sgu_b_in[s], sgu_norm_g[s], sgu_norm_b[s], sgu_w_s[s], sgu_b_s[s], sgu_w_out[s])
            y = _sgu_mixer(h, *sgu_p)
            if ctx_update:
                yc = _sgu_mixer(xc * (1.0 + scc_m) + shc_m, *sgu_p)
        x = _layer_norm(DEEPNORM_ALPHA * x + g_m * y, ln_g[i, 0], ln_b[i, 0])
        moe_p = (router_w[i], router_bias[i], exp_w_gate[i], exp_w_up[i], exp_w_down[i],
                 sh_w_gate[i], sh_w_up[i], sh_w_down[i])
        h = (x * (1.0 + sc_f) + sh_f).reshape(B * N, D)
        if ctx_update:
            xc = _layer_norm(DEEPNORM_ALPHA * xc + gc_m * yc, ln_g[i, 0], ln_b[i, 0])
            hc = (xc * (1.0 + scc_f) + shc_f).reshape(B * C, D)
            yf_all = _moe(jnp.concatenate([h, hc], axis=0), *moe_p)
            yf = yf_all[:B * N].reshape(B, N, D)
            yfc = yf_all[B * N:].reshape(B, C, D)
            xc = _layer_norm(DEEPNORM_ALPHA * xc + gc_f * yfc, ln_g[i, 1], ln_b[i, 1])
        else:
            yf = _moe(h, *moe_p).reshape(B, N, D)
        x = _layer_norm(DEEPNORM_ALPHA * x + g_f * yf, ln_g[i, 1], ln_b[i, 1])
    return x
```

```python
import math
import os
DBG_G = int(os.environ.get('DBG_G', '17'))
DBG_J = int(os.environ.get('DBG_J', '16'))
DBG_V = int(os.environ.get('DBG_V', '1'))
DBG_R = int(os.environ.get('DBG_R', '1'))
DBG_DUMP = int(os.environ.get('DBG_DUMP', '1'))
DBG_W = int(os.environ.get('DBG_W', '1'))
DBG_K = int(os.environ.get('DBG_K', '9'))
DBG_X = int(os.environ.get('DBG_X', '0'))
DBG_M = int(os.environ.get('DBG_M', '9'))
from contextlib import ExitStack

import numpy as np
import concourse.bass as bass
import concourse.mybir as mybir
from concourse.bass_utils import run_bass_kernel_spmd

F32 = mybir.dt.float32
BF16 = mybir.dt.bfloat16
U32 = mybir.dt.uint32
I32 = mybir.dt.int32
ALU = mybir.AluOpType
AF = mybir.ActivationFunctionType
AX = mybir.AxisListType

COMPUTE = ("pe", "act", "dve", "pool")
QUEUES = ("act", "pool", "sp")
NSLOT = 20

D = 1024
NTOK = 4096
NSEQ = 8192
NCTX = 256
NKEY = NSEQ + NCTX
NE = 256
ALPHA = 4.0 ** 0.25
LN_EPS = 1e-5
NBLK = 512
NSLOTS = NBLK * 128


class Buf:
    __slots__ = ("name", "w", "r")

    def __init__(self, name=""):
        self.name = name
        self.w = []
        self.r = {}


class Ins:
    __slots__ = ("fn", "eng", "dma", "deps", "needed", "cnt", "slot", "qidx", "bar")

    def __init__(self, fn, eng, dma):
        self.fn = fn
        self.eng = eng
        self.dma = dma
        self.deps = []
        self.needed = False
        self.cnt = 0
        self.slot = 0
        self.qidx = 0
        self.bar = False


class Prog:
    def __init__(self):
        self.q = {e: [] for e in ("pe", "act", "dve", "pool", "sp")}
        self.ndma = {e: 0 for e in QUEUES}
        self.lastdma = {e: {} for e in QUEUES}
        self.n = 0

    def op(self, eng, fn, reads=(), writes=(), dma=False):
        ins = Ins(fn, eng, dma)
        deps = {}
        raw = set()
        for b in reads:
            for w in b.w:
                deps[id(w)] = w
                raw.add(id(w))
        for b in writes:
            for w in b.w:
                if dma and w.dma:
                    continue
                deps[id(w)] = w
            for r in b.r.values():
                deps[id(r)] = r
        for d in deps.values():
            if d is ins:
                continue
            if d.eng == eng and not d.dma and not dma:
                if eng == "pe" or id(d) not in raw:
                    continue
            ins.deps.append(d)
            d.needed = True
        if dma:
            ins.qidx = self.ndma[eng]
            self.ndma[eng] += 1
            self.lastdma[eng][ins.qidx % NSLOT] = ins
            key = (eng, "d", ins.qidx)
        else:
            key = eng
        for b in reads:
            b.r[key] = ins
        for b in writes:
            if dma and b.w and all(w.dma for w in b.w) and not b.r:
                b.w = b.w + [ins]
            else:
                b.w = [ins]
            b.r = {}
        self.q[eng].append(ins)
        self.n += 1
        return ins

    def pe(self, fn, reads=(), writes=()):
        return self.op("pe", fn, reads, writes)

    def act(self, fn, reads=(), writes=()):
        return self.op("act", fn, reads, writes)

    def dve(self, fn, reads=(), writes=()):
        return self.op("dve", fn, reads, writes)

    def pool(self, fn, reads=(), writes=()):
        return self.op(os.environ.get('POOL_ENG', 'pool'), fn, reads, writes)

    def dma(self, q, fn, reads=(), writes=()):
        return self.op(q, fn, reads, writes, dma=True)

    def barrier(self):
        deps = []
        for e in COMPUTE:
            for ins in reversed(self.q[e]):
                if not ins.dma and not ins.bar:
                    deps.append(ins)
                    ins.needed = True
                    break
        for e in QUEUES:
            deps.extend(self.lastdma[e].values())
        for e in self.q:
            ins = Ins(None, e, False)
            ins.bar = True
            ins.deps = [d for d in deps if not (d.eng == e and not d.dma)]
            self.q[e].append(ins)

    def emit(self, nc, stack):
        esem = {e: stack.enter_context(nc.semaphore("s_" + e)) for e in COMPUTE}
        dsem = {e: [stack.enter_context(nc.semaphore("d_%s_%d" % (e, i))) for i in range(NSLOT)]
                for e in QUEUES}
        for e in COMPUTE:
            c = 0
            for ins in self.q[e]:
                if ins.dma or ins.bar:
                    continue
                if ins.needed:
                    c += 1
                    ins.cnt = c
        for e in QUEUES:
            for ins in self.q[e]:
                if ins.dma:
                    ins.slot = ins.qidx % NSLOT
                    ins.cnt = (ins.qidx // NSLOT + 1) * 16
        block = stack.enter_context(nc.Block())
        prog = self

        def run(ename, eng):
            seen = {}

            def wait(sem, val):
                k = sem.num
                if seen.get(k, -1) >= val:
                    return
                seen[k] = val
                eng.wait_ge(sem, val)

            for ins in prog.q[ename]:
                for d in ins.deps:
                    if d.dma:
                        wait(dsem[d.eng][d.slot], d.cnt)
                    else:
                        wait(esem[d.eng], d.cnt)
                if ins.bar:
                    continue
                if ins.dma:
                    if ins.cnt > 16:
                        wait(dsem[ename][ins.slot], ins.cnt - 16)
                    ins.fn(eng).then_inc(dsem[ename][ins.slot], 16)
                else:
                    r = ins.fn(eng)
                    if ins.needed:
                        r.then_inc(esem[ename], 1)
            if ename in prog.lastdma:
                for sl, ins in prog.lastdma[ename].items():
                    wait(dsem[ename][sl], ins.cnt)

        @block.tensor
        def _(eng):
            run("pe", eng)

        @block.scalar
        def _(eng):
            run("act", eng)

        @block.vector
        def _(eng):
            run("dve", eng)

        @block.gpsimd
        def _(eng):
            run("pool", eng)

        @block.sync
        def _(eng):
            run("sp", eng)


class Arena:
    def __init__(self, nc, st, nbytes):
        self.t = st.enter_context(nc.sbuf_tensor("arena", [128, nbytes // 4], F32))
        self.off = 0
        self.cap = nbytes

    def alloc(self, shape, dtype):
        sz = 2 if dtype == BF16 else 4
        nelem = int(np.prod(shape))
        nb = (nelem * sz + 63) // 64 * 64
        a = self.off
        self.off += nb
        assert self.off <= self.cap, ("arena overflow", self.off, self.cap)
        v = self.t[:, a // 4:(a + nb) // 4]
        if dtype != F32:
            v = v.bitcast(dtype)
        v = v[:, :nelem]
        if len(shape) == 2:
            v = v.rearrange("p (a b) -> p a b", a=shape[0])
        elif len(shape) == 3:
            v = v.rearrange("p (a b c) -> p a b c", a=shape[0], b=shape[1])
        return v


def build(stop_after="D"):
    nc = bass.Bass("TRN2", target_bir_lowering=False)
    P = Prog()

    def din(name, shape, dtype=F32):
        return nc.dram_tensor(name, list(shape), dtype, kind="ExternalInput").ap()

    def dscr(name, shape, dtype):
        return nc.dram_tensor(name, list(shape), dtype, kind="Internal").ap()

    x_in = din("x", [NSEQ, D])
    ctx_in = din("ctx", [NCTX, D])
    c_bc_in = din("c_bc", [128, D])
    cc_bc_in = din("cc_bc", [128, D])
    w_mod_in = din("w_mod", [2, D, 6 * D])
    b_mod_in = din("b_mod_bc", [2, 128, 6 * D])
    ln_in = din("ln_bc", [2, 2, 2, 128, D])
    w_in_in = din("attn_w_in", [D, 3 * D])
    w_out_in = din("attn_w_out", [D, D])
    lam_in = din("lam_bc", [128, 4 * 64])
    subln_in = din("subln_col", [128, 1])
    cos_in = din("cosT", [128, NSEQ])
    sin_in = din("sinT", [128, NSEQ])
    cst_in = din("cst", [128, 5, 128])
    iota_in = din("iota", [128, 256 + 512 + 1])
    router_w_in = din("router_w", [2, D, NE])
    rbias_in = din("rbias_bc", [2, 128, NE])
    exp_g_in = din("exp_w_gate", [2 * NE * 128, 2048])
    exp_u_in = din("exp_w_up", [2 * NE * 128, 2048])
    exp_d_in = din("exp_w_down", [2 * NE * 128, 2048])
    sh_g_in = din("sh_w_gate", [2, D, 256])
    sh_u_in = din("sh_w_up", [2, D, 256])
    sh_d_in = din("sh_w_down", [2, 256, D])
    sgu_w_in_in = din("sgu_w_in", [D, 4096])
    sgu_w_out_in = din("sgu_w_out", [2048, D])
    sgu_ngb_in = din("sgu_ngb", [2, 128, 2048])
    sgu_binv_in = din("sgu_binv", [128, 2048])
    sgu_binu_in = din("sgu_binu", [128, 16])
    sgu_bs16_in = din("sgu_bs16", [128, 2048])
    sgu_wsT_in = din("sgu_wsT", [128, 8, 128])
    out_d = nc.dram_tensor("out", [NTOK, D], F32, kind="ExternalOutput").ap()

    KT_d = dscr("KT_d", [8, 128, NKEY], BF16)
    V_d = dscr("V_d", [NKEY, D], BF16)
    QT_d = dscr("QT_d", [8, 128, NTOK], BF16)
    X1_d = dscr("X1_d", [NTOK, D], F32)
    X2_d = dscr("X2_d", [NTOK, D], F32)
    X3_d = dscr("X3_d", [NTOK, D], F32)
    XS_d = dscr("XS_d", [NSLOTS, D], BF16)
    YS_d = dscr("YS_d", [NSLOTS, D], BF16)
    TT_d = dscr("TT_d", [32, 128, 2048], BF16)
    B_TT = Buf("TT")
    B_KT, B_V, B_QT = Buf("KT_d"), Buf("V_d"), Buf("QT_d")
    B_X1, B_X2, B_X3, B_OUT = Buf("X1"), Buf("X2"), Buf("X3"), Buf("OUT")

    with ExitStack() as st:
        AR = Arena(nc, st, 204 * 1024)
        psum = st.enter_context(nc.psum_tensor("psum", [128, 8, 512], F32))
        PSB = [Buf("ps%d" % i) for i in range(8)]

        def bank(i):
            return psum[:, i, :]

        def bank_bf(i):
            return psum[:, i, :].bitcast(BF16)

        cst_f = AR.alloc([5, 128], F32)
        cst_b = AR.alloc([5, 128], BF16)
        iota = AR.alloc([256 + 512 + 1], F32)
        B_cst = Buf("cst")
        P.dma("sp", lambda e: e.dma_start(out=cst_f, in_=cst_in), writes=[B_cst])
        P.dma("pool", lambda e: e.dma_start(out=cst_b, in_=cst_in), writes=[B_cst])
        P.dma("sp", lambda e: e.dma_start(out=iota, in_=iota_in), writes=[B_cst])
        ident_f, ones_f, onesd_f = cst_f[:, 0, :], cst_f[:, 1, :], cst_f[:, 4, :]
        ident_b, ones_b, ustr_b, perm_b = cst_b[:, 0, :], cst_b[:, 1, :], cst_b[:, 2, :], cst_b[:, 3, :]
        iota_e = iota[:, 0:256]
        iota_blk = iota[:, 256:768]
        iota_p = iota[:, 768:769]

        modbc = AR.alloc([6, D], F32)
        modcc = AR.alloc([2, D], F32)
        lnbc = AR.alloc([4, D], F32)
        B_mod, B_modc, B_ln = Buf("mod"), Buf("modc"), Buf("ln")
        persist_mark = AR.off

        def phase_mod(l, with_ctx):
            AR.off = persist_mark
            cb = AR.alloc([D], F32)
            sb = AR.alloc([D], F32)
            sT = AR.alloc([8, 128], F32)
            bmod = AR.alloc([6 * D], F32)
            wblk = [AR.alloc([8, 512], F32) for _ in range(2)]
            B_cb, B_sb, B_sT, B_bm = Buf(), Buf(), Buf(), Buf()
            B_w = [Buf(), Buf()]
            P.dma("sp", lambda e: e.dma_start(out=bmod, in_=b_mod_in[l]), writes=[B_bm])
            P.dma("act", lambda e: e.dma_start(out=lnbc, in_=ln_in[l].rearrange("a b p d -> p (a b) d")),
                  writes=[B_ln])
            srcs = [(c_bc_in, modbc, B_mod, 12)]
            if with_ctx:
                srcs.append((cc_bc_in, modcc, B_modc, 4))
            blk = 0
            for (src, dst, B_dst, nblk) in srcs:
                P.dma("sp", lambda e, src=src: e.dma_start(out=cb, in_=src), writes=[B_cb])
                P.act(lambda e: e.activation(out=sb, in_=cb, func=AF.Silu), reads=[B_cb], writes=[B_sb])
                for k in range(8):
                    P.pe(lambda e, k=k: e.transpose(out=bank(0)[:, k * 128:(k + 1) * 128] if k < 4 else
                                                     bank(1)[:, (k - 4) * 128:(k - 3) * 128],
                                                     in_=sb[:, k * 128:(k + 1) * 128], identity=ident_f),
                         reads=[B_sb, B_cst], writes=[PSB[k // 4]])
                P.dve(lambda e: e.tensor_copy(out=sT[:, 0:4, :], in_=bank(0).rearrange("p (a b) -> p a b", a=4)),
                      reads=[PSB[0]], writes=[B_sT])
                P.dve(lambda e: e.tensor_copy(out=sT[:, 4:8, :], in_=bank(1).rearrange("p (a b) -> p a b", a=4)),
                      reads=[PSB[1]], writes=[B_sT])
                dflat = dst.rearrange("p a b -> p (a b)")
                for n in range(nblk):
                    wb, Bw = wblk[blk % 2], B_w[blk % 2]
                    pb = 2 + blk % 2
                    blk += 1
                    P.dma("sp", lambda e, n=n, wb=wb: e.dma_start(
                        out=wb, in_=w_mod_in[l][:, n * 512:(n + 1) * 512].rearrange("(k p) n -> p k n", p=128)),
                        writes=[Bw])
                    for k in range(8):
                        P.pe(lambda e, k=k, wb=wb, pb=pb: e.matmul(bank(pb), sT[:, k, :], wb[:, k, :],
                                                                   start=(k == 0), stop=(k == 7)),
                             reads=[B_sT, Bw], writes=[PSB[pb]])
                    P.dve(lambda e, n=n, pb=pb, dflat=dflat: e.tensor_tensor(
                        out=dflat[:, n * 512:(n + 1) * 512], in0=bank(pb), in1=bmod[:, n * 512:(n + 1) * 512],
                        op=ALU.add), reads=[PSB[pb], B_bm], writes=[B_dst])
                for ch in ((1, 4) if nblk == 12 else (1,)):
                    P.dve(lambda e, ch=ch, dst=dst: e.tensor_scalar_add(out=dst[:, ch, :], in0=dst[:, ch, :],
                                                                       scalar1=1.0),
                          reads=[B_dst], writes=[B_dst])
            P.barrier()

        def layer_norm_store(zt, B_z, gi, out_ap, B_out, tmp, B_tmp, st4, B_st, q="sp"):
            mean, var, rstd, nmean = st4[:, 0:1], st4[:, 1:2], st4[:, 2:3], st4[:, 3:4]
            P.dve(lambda e: e.tensor_reduce(out=mean, in_=zt, axis=AX.X, op=ALU.add), reads=[B_z], writes=[B_st])
            P.dve(lambda e: e.tensor_scalar_mul(out=nmean, in0=mean, scalar1=-1.0 / D), reads=[B_st], writes=[B_st])
            P.act(lambda e: e.activation(out=zt, in_=zt, func=AF.Identity, bias=nmean, scale=1.0),
                  reads=[B_z, B_st], writes=[B_z])
            P.act(lambda e: e.activation(out=tmp, in_=zt, func=AF.Square, accum_out=var),
                  reads=[B_z], writes=[B_tmp, B_st])
            P.act(lambda e: e.activation(out=rstd, in_=var, func=AF.Sqrt, bias=eps_col, scale=1.0 / D),
                  reads=[B_st, B_cst2], writes=[B_st])
            P.dve(lambda e: e.reciprocal(out=rstd, in_=rstd), reads=[B_st], writes=[B_st])
            P.dve(lambda e: e.scalar_tensor_tensor(out=zt, in0=zt, scalar=rstd, in1=lnbc[:, 2 * gi, :],
                                                   op0=ALU.mult, op1=ALU.mult),
                  reads=[B_z, B_st, B_ln], writes=[B_z])
            P.pool(lambda e: e.tensor_tensor(out=zt, in0=zt, in1=lnbc[:, 2 * gi + 1, :], op=ALU.add),
                   reads=[B_z, B_ln], writes=[B_z])
            P.dma(q, lambda e: e.dma_start(out=out_ap, in_=zt), reads=[B_z], writes=[B_out])

        eps_col = AR.alloc([1], F32)
        B_cst2 = Buf()
        P.dve(lambda e: e.memset(eps_col, LN_EPS), writes=[B_cst2])
        persist_mark = AR.off

        def make_hT(xt, B_x, sc, sh, B_m, hb, B_hb, pb, hT_dst, B_hT):
            P.dve(lambda e: e.tensor_tensor(out=xt, in0=xt, in1=sc, op=ALU.mult), reads=[B_x, B_m], writes=[B_x])
            P.dve(lambda e: e.tensor_tensor(out=hb, in0=xt, in1=sh, op=ALU.add), reads=[B_x, B_m], writes=[B_hb])
            pt = bank_bf(pb)
            for k in range(8):
                P.pe(lambda e, k=k: e.transpose(out=pt[:, k * 128:(k + 1) * 128], in_=hb[:, k * 128:(k + 1) * 128],
                                                identity=ident_b), reads=[B_hb, B_cst], writes=[PSB[pb]])
            P.act(lambda e: e.copy(out=hT_dst, in_=pt.rearrange("p (a b) -> p a b", a=8)),
                  reads=[PSB[pb]], writes=[B_hT])

        def phase_attn():
            AR.off = persist_mark
            m0 = AR.off
            w_in = AR.alloc([8, 3 * D], BF16)
            B_win = Buf()
            for k in range(8 if DBG_W else 0):
                for c3 in range(3):
                    P.dma("pool", lambda e, k=k, c3=c3: e.dma_start(out=w_in[:, k, c3 * D:(c3 + 1) * D],
                                                                   in_=w_in_in[k * 128:(k + 1) * 128, c3 * D:(c3 + 1) * D]),
                          writes=[B_win])
            xt = [AR.alloc([D], F32) for _ in range(2)]
            hb = [AR.alloc([D], BF16) for _ in range(2)]
            hT = [AR.alloc([8, 512], BF16) for _ in range(2)]
            cs = [AR.alloc([2, 512], F32) for _ in range(2)]
            kraw = [AR.alloc([512], BF16) for _ in range(2)]
            t1 = [AR.alloc([512], F32) for _ in range(2)]
            t2 = [AR.alloc([512], F32) for _ in range(2)]
            kout = [AR.alloc([512], BF16) for _ in range(2)]
            vb = [AR.alloc([D], BF16) for _ in range(2)]
            B_xt, B_hb, B_hT, B_cs = [Buf(), Buf()], [Buf(), Buf()], [Buf(), Buf()], [Buf(), Buf()]
            B_kraw, B_t1, B_t2, B_kout, B_vb = ([Buf(), Buf()] for _ in range(5))
            ti = 0
            ci = 0
            for g in (list(range(17)) if DBG_G == 17 else list(range(DBG_G))):
                ctxg = (g == 16)
                ntile = 2 if ctxg else 4
                N = ntile * 128
                gb = g % 2
                if not ctxg:
                    P.dma("sp", lambda e, g=g, gb=gb: e.dma_start(out=cs[gb][:, 0, :], in_=cos_in[:, g * 512:(g + 1) * 512]),
                          writes=[B_cs[gb]])
                    P.dma("sp", lambda e, g=g, gb=gb: e.dma_start(out=cs[gb][:, 1, :], in_=sin_in[:, g * 512:(g + 1) * 512]),
                          writes=[B_cs[gb]])
                for t in range(ntile):
                    b2 = ti % 2
                    ti += 1
                    src = ctx_in[t * 128:(t + 1) * 128, :] if ctxg else x_in[g * 512 + t * 128:g * 512 + (t + 1) * 128, :]
                    P.dma("sp", lambda e, b2=b2, src=src: e.dma_start(out=xt[b2], in_=src), writes=[B_xt[b2]])
                    sc = modcc[:, 1, :] if ctxg else modbc[:, 1, :]
                    sh = modcc[:, 0, :] if ctxg else modbc[:, 0, :]
                    make_hT(xt[b2], B_xt[b2], sc, sh, B_modc if ctxg else B_mod, hb[b2], B_hb[b2], b2,
                            hT[gb][:, :, t * 128:(t + 1) * 128], B_hT[gb])
                jobs = [("k", j) for j in range(8)]
                if g < 8:
                    jobs += [("q", j) for j in range(8)]
                for (kind, j) in jobs[:DBG_J]:
                    c2 = ci % 2
                    ci += 1
                    pa, pbk = 2 + c2, 4 + c2
                    col0 = (D if kind == "k" else 0) + j * 128
                    for k in range(8):
                        P.pe(lambda e, k=k, pa=pa, col0=col0, gb=gb, N=N: e.matmul(
                            bank(pa)[:, :N], w_in[:, k, col0:col0 + 128], hT[gb][:, k, :N], start=(k == 0), stop=(k == 7)),
                            reads=[B_win, B_hT[gb]], writes=[PSB[pa]])
                    if DBG_K < 2:
                        continue
                    P.act(lambda e, c2=c2, pa=pa, N=N: e.copy(out=kraw[c2][:, :N], in_=bank(pa)[:, :N]),
                          reads=[PSB[pa]], writes=[B_kraw[c2]])
                    if DBG_K < 3:
                        continue
                    if ctxg or not DBG_R:
                        P.dma("sp", lambda e, c2=c2, j=j: e.dma_start(out=KT_d[j, :, NSEQ:NKEY], in_=kraw[c2][:, :NCTX]),
                              reads=[B_kraw[c2]], writes=[B_KT])
                        continue
                    P.pe(lambda e, c2=c2, pbk=pbk: e.matmul(bank(pbk), perm_b, kraw[c2], start=True, stop=True),
                         reads=[B_cst, B_kraw[c2]], writes=[PSB[pbk]])
                    if DBG_K < 4:
                        continue
                    P.dve(lambda e, c2=c2, pa=pa, gb=gb: e.tensor_tensor(out=t1[c2], in0=(kraw[c2] if DBG_X == 1 else bank(pa)), in1=(t2[c2] if DBG_X == 2 else cs[gb][:, 0, :]), op=ALU.mult),
                          reads=[PSB[pa], B_cs[gb], B_kraw[c2]], writes=[B_t1[c2]])
                    P.dve(lambda e, c2=c2, pbk=pbk, gb=gb: e.tensor_tensor(out=t2[c2], in0=bank(pbk), in1=cs[gb][:, 1, :], op=ALU.mult),
                          reads=[PSB[pbk], B_cs[gb]], writes=[B_t2[c2]])
                    if DBG_K < 5:
                        continue
                    P.pool(lambda e, c2=c2: e.tensor_tensor(out=kout[c2], in0=t1[c2], in1=t2[c2], op=ALU.add),
                           reads=[B_t1[c2], B_t2[c2]], writes=[B_kout[c2]])
                    if kind == "k":
                        P.dma("sp", lambda e, c2=c2, j=j, g=g: e.dma_start(out=KT_d[j, :, g * 512:(g + 1) * 512], in_=kout[c2]),
                              reads=[B_kout[c2]], writes=[B_KT])
                    else:
                        P.dma("sp", lambda e, c2=c2, j=j, g=g: e.dma_start(out=QT_d[j, :, g * 512:(g + 1) * 512], in_=kout[c2]),
                              reads=[B_kout[c2]], writes=[B_QT])
                for t in range(ntile if DBG_V else 0):
                    v2 = t % 2
                    for c in range(2):
                        for k in range(8):
                            P.pe(lambda e, k=k, c=c, t=t, gb=gb: e.matmul(
                                bank(6 + c), hT[gb][:, k, t * 128:(t + 1) * 128], w_in[:, k, 2 * D + c * 512:2 * D + (c + 1) * 512],
                                start=(k == 0), stop=(k == 7)), reads=[B_win, B_hT[gb]], writes=[PSB[6 + c]])
                    P.dve(lambda e, v2=v2: e.tensor_copy(out=vb[v2][:, 0:512], in_=bank(6)), reads=[PSB[6]], writes=[B_vb[v2]])
                    P.act(lambda e, v2=v2: e.copy(out=vb[v2][:, 512:1024], in_=bank(7)), reads=[PSB[7]], writes=[B_vb[v2]])
                    row0 = NSEQ + t * 128 if ctxg else g * 512 + t * 128
                    P.dma("act", lambda e, v2=v2, row0=row0: e.dma_start(out=V_d[row0:row0 + 128, :], in_=vb[v2]),
                          reads=[B_vb[v2]], writes=[B_V])
            P.barrier()
            if stop_after == "A1":
                AR.off = m0
                qb = AR.alloc([NTOK], BF16)
                qf = AR.alloc([NTOK], F32)
                Bq, Bf = Buf(), Buf()
                for j in range(8 if DBG_DUMP else 0):
                    P.dma("sp", lambda e, j=j: e.dma_start(out=qb, in_=QT_d[j]), reads=[B_QT], writes=[Bq])
                    P.dve(lambda e: e.tensor_copy(out=qf, in_=qb), reads=[Bq], writes=[Bf])
                    for c in range(4):
                        P.dma("sp", lambda e, j=j, c=c: e.dma_start(out=out_d[(j * 4 + c) * 128:(j * 4 + c + 1) * 128, :],
                                                                  in_=qf[:, c * 1024:(c + 1) * 1024]), reads=[Bf], writes=[B_OUT])
                return

            AR.off = m0
            onT = AR.alloc([8, NTOK], BF16)
            B_onT = Buf()
            m_on = AR.off
            KT = [AR.alloc([NKEY], BF16)] * 2
            Vh = [AR.alloc([66, 128], BF16)] * 2
            QT = [AR.alloc([NTOK], BF16)] * 2
            B_KTs, B_Vh, B_QTs = [Buf()] * 2, [Buf()] * 2, [Buf()] * 2
            E = [AR.alloc([512], BF16) for _ in range(4)]
            B_E = [Buf() for _ in range(4)]
            Es = [AR.alloc([512], F32) for _ in range(2)]
            B_Es = [Buf(), Buf()]
            r12 = [AR.alloc([512], F32) for _ in range(2)]
            B_r = [Buf(), Buf()]
            oT = AR.alloc([512], F32)
            o2 = AR.alloc([512], F32)
            sq = AR.alloc([512], F32)
            B_oT, B_o2, B_sq = Buf(), Buf(), Buf()
            lamt = AR.alloc([4 * 64], F32)
            lamw = AR.alloc([2 * 64], F32)
            lams = AR.alloc([4], F32)
            gcol = AR.alloc([1], F32)
            B_lam = Buf()
            P.dma("sp", lambda e: e.dma_start(out=lamt, in_=lam_in), writes=[B_lam])
            P.dma("sp", lambda e: e.dma_start(out=gcol, in_=subln_in), writes=[B_lam])
            P.dve(lambda e: e.tensor_tensor(out=lamw[:, 0:64], in0=lamt[:, 0:64], in1=lamt[:, 64:128], op=ALU.mult),
                  reads=[B_lam], writes=[B_lam])
            P.dve(lambda e: e.tensor_tensor(out=lamw[:, 64:128], in0=lamt[:, 128:192], in1=lamt[:, 192:256], op=ALU.mult),
                  reads=[B_lam], writes=[B_lam])
            P.dve(lambda e: e.tensor_reduce(out=lams[:, 0:1], in_=lamw[:, 0:64], axis=AX.X, op=ALU.add), reads=[B_lam], writes=[B_lam])
            P.dve(lambda e: e.tensor_reduce(out=lams[:, 1:2], in_=lamw[:, 64:128], axis=AX.X, op=ALU.add), reads=[B_lam], writes=[B_lam])
            P.act(lambda e: e.activation(out=lams[:, 0:2], in_=lams[:, 0:2], func=AF.Exp), reads=[B_lam], writes=[B_lam])
            P.dve(lambda e: e.tensor_tensor(out=lams[:, 2:3], in0=lams[:, 1:2], in1=lams[:, 0:1], op=ALU.subtract), reads=[B_lam], writes=[B_lam])
            P.dve(lambda e: e.tensor_scalar_add(out=lams[:, 3:4], in0=lams[:, 2:3], scalar1=-0.2), reads=[B_lam], writes=[B_lam])
            neg_lam = lams[:, 3:4]
            P.dve(lambda e: e.tensor_scalar_mul(out=gcol, in0=gcol, scalar1=0.8), reads=[B_lam], writes=[B_lam])

            for j in range(8):
                hb2 = j % 2
                P.dma("sp", lambda e, j=j, hb2=hb2: e.dma_start(out=KT[hb2], in_=KT_d[j]), reads=[B_KT], writes=[B_KTs[hb2]])
                P.dma("act", lambda e, j=j, hb2=hb2: e.dma_start(
                    out=Vh[hb2], in_=V_d[:, j * 128:(j + 1) * 128].rearrange("(t p) e -> p t e", p=128)),
                    reads=[B_V], writes=[B_Vh[hb2]])
                P.dma("sp", lambda e, j=j, hb2=hb2: e.dma_start(out=QT[hb2], in_=QT_d[j]), reads=[B_QT], writes=[B_QTs[hb2]])
                for g in range(8):
                    for kt in range(66):
                        sb2 = kt % 2
                        for m in range(2):
                            pbk = 2 * sb2 + m
                            ei = 2 * sb2 + m
                            P.pe(lambda e, m=m, pbk=pbk, kt=kt, g=g, hb2=hb2: e.matmul(
                                bank(pbk), KT[hb2][m * 64:(m + 1) * 64, kt * 128:(kt + 1) * 128],
                                QT[hb2][m * 64:(m + 1) * 64, g * 512:(g + 1) * 512], start=True, stop=True),
                                reads=[B_KTs[hb2], B_QTs[hb2]], writes=[PSB[pbk]])
                            P.act(lambda e, pbk=pbk, ei=ei: e.activation(out=E[ei], in_=bank(pbk), func=AF.Exp, scale=0.125),
                                  reads=[PSB[pbk]], writes=[B_E[ei]])
                            P.pe(lambda e, m=m, ei=ei, kt=kt, hb2=hb2: e.matmul(
                                bank(4 + m), Vh[hb2][:, kt, :], E[ei], start=(kt == 0), stop=(kt == 65)),
                                reads=[B_Vh[hb2], B_E[ei]], writes=[PSB[4 + m]])
                            if kt == 0:
                                P.pool(lambda e, m=m, ei=ei: e.tensor_copy(out=Es[m], in_=E[ei]),
                                       reads=[B_E[ei]], writes=[B_Es[m]])
                            else:
                                P.pool(lambda e, m=m, ei=ei: e.tensor_tensor(out=Es[m], in0=Es[m], in1=E[ei], op=ALU.add),
                                       reads=[B_E[ei], B_Es[m]], writes=[B_Es[m]])
                    for m in range(2):
                        P.pe(lambda e, m=m: e.matmul(bank(6), ones_f, Es[m], start=True, stop=True),
                             reads=[B_cst, B_Es[m]], writes=[PSB[6]])
                        P.dve(lambda e, m=m: e.reciprocal(out=r12[m], in_=bank(6)), reads=[PSB[6]], writes=[B_r[m]])
                    P.dve(lambda e: e.tensor_tensor(out=oT, in0=bank(4), in1=r12[0], op=ALU.mult),
                          reads=[PSB[4], B_r[0]], writes=[B_oT])
                    P.dve(lambda e: e.tensor_tensor(out=o2, in0=bank(5), in1=r12[1], op=ALU.mult),
                          reads=[PSB[5], B_r[1]], writes=[B_o2])
                    P.dve(lambda e: e.scalar_tensor_tensor(out=oT, in0=o2, scalar=neg_lam, in1=oT, op0=ALU.mult, op1=ALU.add),
                          reads=[B_o2, B_oT, B_lam], writes=[B_oT])
                    P.pool(lambda e: e.tensor_tensor(out=sq, in0=oT, in1=oT, op=ALU.mult), reads=[B_oT], writes=[B_sq])
                    P.pe(lambda e: e.matmul(bank(7), onesd_f, sq, start=True, stop=True),
                         reads=[B_cst, B_sq], writes=[PSB[7]])
                    P.act(lambda e: e.activation(out=o2, in_=bank(7), func=AF.Sqrt, bias=eps_col, scale=1.0),
                          reads=[PSB[7], B_cst2], writes=[B_o2])
                    P.dve(lambda e: e.reciprocal(out=o2, in_=o2), reads=[B_o2], writes=[B_o2])
                    P.dve(lambda e, j=j, g=g: e.scalar_tensor_tensor(
                        out=onT[:, j, g * 512:(g + 1) * 512], in0=oT, scalar=gcol, in1=o2, op0=ALU.mult, op1=ALU.mult),
                        reads=[B_oT, B_o2, B_lam], writes=[B_onT])
            P.barrier()

            AR.off = m_on
            w_o = AR.alloc([8, D], BF16)
            B_wo = Buf()
            for k in range(8):
                P.dma("pool", lambda e, k=k: e.dma_start(out=w_o[:, k, :], in_=w_out_in[k * 128:(k + 1) * 128, :]),
                      writes=[B_wo])
            xr = [AR.alloc([D], F32) for _ in range(2)]
            zt = [AR.alloc([D], F32) for _ in range(2)]
            tmp = AR.alloc([D], F32)
            st4 = [AR.alloc([4], F32) for _ in range(2)]
            B_xr, B_zt, B_st = [Buf(), Buf()], [Buf(), Buf()], [Buf(), Buf()]
            B_tmp = Buf()
            for t in range(32):
                b2 = t % 2
                P.dma("sp", lambda e, t=t, b2=b2: e.dma_start(out=xr[b2], in_=x_in[t * 128:(t + 1) * 128, :]), writes=[B_xr[b2]])
                for c in range(2):
                    pbk = 2 * b2 + c
                    for j in range(8):
                        P.pe(lambda e, j=j, c=c, t=t, pbk=pbk: e.matmul(
                            bank(pbk), onT[:, j, t * 128:(t + 1) * 128], w_o[:, j, c * 512:(c + 1) * 512],
                            start=(j == 0), stop=(j == 7)), reads=[B_onT, B_wo], writes=[PSB[pbk]])
                    P.dve(lambda e, c=c, b2=b2, pbk=pbk: e.tensor_tensor(
                        out=zt[b2][:, c * 512:(c + 1) * 512], in0=bank(pbk), in1=modbc[:, 2, c * 512:(c + 1) * 512], op=ALU.mult),
                        reads=[PSB[pbk], B_mod], writes=[B_zt[b2]])
                P.dve(lambda e, b2=b2: e.scalar_tensor_tensor(out=zt[b2], in0=xr[b2], scalar=ALPHA, in1=zt[b2],
                                                              op0=ALU.mult, op1=ALU.add),
                      reads=[B_xr[b2], B_zt[b2]], writes=[B_zt[b2]])
                layer_norm_store(zt[b2], B_zt[b2], 0, X1_d[t * 128:(t + 1) * 128, :], B_X1, tmp, B_tmp, st4[b2], B_st[b2])
            P.barrier()

        def phase_sgu(Xin_d, B_in, Xout_d, B_out):
            AR.off = persist_mark
            m0 = AR.off
            w1 = AR.alloc([8, 4096], BF16)
            B_w1 = Buf()
            for k in range(8):
                for c4 in range(4):
                    P.dma("pool", lambda e, k=k, c4=c4: e.dma_start(out=w1[:, k, c4 * D:(c4 + 1) * D],
                                                                   in_=sgu_w_in_in[k * 128:(k + 1) * 128, c4 * D:(c4 + 1) * D]),
                          writes=[B_w1])
            ngb = AR.alloc([2, 2048], F32)
            binv = AR.alloc([2048], F32)
            binu = AR.alloc([16], F32)
            bs16 = AR.alloc([2048], F32)
            wsT = AR.alloc([8, 128], BF16)
            B_cc = Buf()
            P.dma("sp", lambda e: e.dma_start(out=ngb, in_=sgu_ngb_in.rearrange("a p d -> p a d")), writes=[B_cc])
            P.dma("sp", lambda e: e.dma_start(out=binv, in_=sgu_binv_in), writes=[B_cc])
            P.dma("sp", lambda e: e.dma_start(out=binu, in_=sgu_binu_in), writes=[B_cc])
            P.dma("sp", lambda e: e.dma_start(out=bs16, in_=sgu_bs16_in), writes=[B_cc])
            P.dma("pool", lambda e: e.dma_start(out=wsT, in_=sgu_wsT_in), writes=[B_cc])
            xt = [AR.alloc([D], F32) for _ in range(2)]
            hb = AR.alloc([D], BF16)
            hT = AR.alloc([8, 128], BF16)
            uT = AR.alloc([2048], BF16)
            vz = AR.alloc([2048], F32)
            vn = AR.alloc([2048], BF16)
            tT = [AR.alloc([2048], BF16) for _ in range(2)]
            st4 = AR.alloc([4], F32)
            B_xt, B_tT = [Buf(), Buf()], [Buf(), Buf()]
            B_hb, B_hT, B_uT, B_vz, B_vn, B_st = Buf(), Buf(), Buf(), Buf(), Buf(), Buf()
            mean, var, rstd, nmean = st4[:, 0:1], st4[:, 1:2], st4[:, 2:3], st4[:, 3:4]
            ps4a = psum[:, 0:4, :].rearrange("p a b -> p (a b)")
            ps4b = psum[:, 4:8, :].rearrange("p a b -> p (a b)")
            B_pa, B_pb = Buf(), Buf()
            for t in range(32):
                b2 = t % 2
                P.dma("sp", lambda e, t=t, b2=b2: e.dma_start(out=xt[b2], in_=Xin_d[t * 128:(t + 1) * 128, :]),
                      reads=[B_in], writes=[B_xt[b2]])
                P.dve(lambda e, b2=b2: e.tensor_tensor(out=xt[b2], in0=xt[b2], in1=modbc[:, 1, :], op=ALU.mult),
                      reads=[B_xt[b2], B_mod], writes=[B_xt[b2]])
                P.dve(lambda e, b2=b2: e.tensor_tensor(out=hb, in0=xt[b2], in1=modbc[:, 0, :], op=ALU.add),
                      reads=[B_xt[b2], B_mod], writes=[B_hb])
                pt = bank_bf(7)
                for k in range(8):
                    P.pe(lambda e, k=k: e.transpose(out=pt[:, k * 128:(k + 1) * 128], in_=hb[:, k * 128:(k + 1) * 128],
                                                    identity=ident_b), reads=[B_hb, B_cst], writes=[B_pb])
                P.act(lambda e: e.copy(out=hT, in_=pt.rearrange("p (a b) -> p a b", a=8)), reads=[B_pb], writes=[B_hT])
                for ch in range(16):
                    for k in range(8):
                        P.pe(lambda e, ch=ch, k=k: e.matmul(ps4a[:, ch * 128:(ch + 1) * 128], w1[:, k, ch * 128:(ch + 1) * 128],
                                                            hT[:, k, :], start=(k == 0), stop=(k == 7)),
                             reads=[B_w1, B_hT], writes=[B_pa])
                for ch in range(16):
                    P.act(lambda e, ch=ch: e.activation(out=uT[:, ch * 128:(ch + 1) * 128], in_=ps4a[:, ch * 128:(ch + 1) * 128],
                                                        func=AF.Gelu, bias=binu[:, ch:ch + 1], scale=1.0),
                          reads=[B_pa, B_cc], writes=[B_uT])
                for c in range(4):
                    for k in range(8):
                        P.pe(lambda e, c=c, k=k: e.matmul(ps4b[:, c * 512:(c + 1) * 512], hT[:, k, :],
                                                          w1[:, k, 2048 + c * 512:2048 + (c + 1) * 512], start=(k == 0), stop=(k == 7)),
                             reads=[B_w1, B_hT], writes=[B_pb])
                P.dve(lambda e: e.tensor_tensor(out=vz, in0=ps4b, in1=binv, op=ALU.add), reads=[B_pb, B_cc], writes=[B_vz])
                P.act(lambda e: e.activation(out=vz, in_=vz, func=AF.Gelu), reads=[B_vz], writes=[B_vz])
                P.dve(lambda e: e.tensor_reduce(out=mean, in_=vz, axis=AX.X, op=ALU.add), reads=[B_vz], writes=[B_st])
                P.dve(lambda e: e.tensor_scalar_mul(out=nmean, in0=mean, scalar1=-1.0 / 2048), reads=[B_st], writes=[B_st])
                P.act(lambda e: e.activation(out=vz, in_=vz, func=AF.Identity, bias=nmean, scale=1.0),
                      reads=[B_vz, B_st], writes=[B_vz])
                P.act(lambda e: e.activation(out=vn, in_=vz, func=AF.Square, accum_out=var), reads=[B_vz], writes=[B_vn, B_st])
                P.act(lambda e: e.activation(out=rstd, in_=var, func=AF.Sqrt, bias=eps_col, scale=1.0 / 2048),
                      reads=[B_st, B_cst2], writes=[B_st])
                P.dve(lambda e: e.reciprocal(out=rstd, in_=rstd), reads=[B_st], writes=[B_st])
                P.dve(lambda e: e.scalar_tensor_tensor(out=vz, in0=vz, scalar=rstd, in1=ngb[:, 0, :], op0=ALU.mult, op1=ALU.mult),
                      reads=[B_vz, B_st, B_cc], writes=[B_vz])
                P.dve(lambda e: e.tensor_tensor(out=vn, in0=vz, in1=ngb[:, 1, :], op=ALU.add), reads=[B_vz, B_cc, B_vn], writes=[B_vn])
                for ch in range(16):
                    P.pe(lambda e, ch=ch: e.matmul(ps4a[:, ch * 128:(ch + 1) * 128], vn[:, ch * 128:(ch + 1) * 128],
                                                   wsT[:, ch // 2, :], start=True, stop=True),
                         reads=[B_vn, B_cc], writes=[B_pa])
                P.dve(lambda e: e.tensor_tensor(out=vz, in0=ps4a, in1=bs16, op=ALU.add), reads=[B_pa, B_cc, B_vz], writes=[B_vz])
                P.dve(lambda e, b2=b2: e.tensor_tensor(out=tT[b2], in0=vz, in1=uT, op=ALU.mult), reads=[B_vz, B_uT], writes=[B_tT[b2]])
                P.dma("act", lambda e, t=t, b2=b2: e.dma_start(out=TT_d[t], in_=tT[b2]), reads=[B_tT[b2]], writes=[B_TT])
            P.barrier()
            AR.off = m0
            w2 = AR.alloc([16, D], BF16)
            B_w2 = Buf()
            for k in range(16):
                P.dma("pool", lambda e, k=k: e.dma_start(out=w2[:, k, :], in_=sgu_w_out_in[k * 128:(k + 1) * 128, :]),
                      writes=[B_w2])
            tl = [AR.alloc([2048], BF16) for _ in range(2)]
            xr = [AR.alloc([D], F32) for _ in range(2)]
            zt = [AR.alloc([D], F32) for _ in range(2)]
            tmp = AR.alloc([D], F32)
            s4 = [AR.alloc([4], F32) for _ in range(2)]
            B_tl, B_xr, B_zt, B_s4 = ([Buf(), Buf()] for _ in range(4))
            B_tmp = Buf()
            for t in range(32):
                b2 = t % 2
                P.dma("sp", lambda e, t=t, b2=b2: e.dma_start(out=xr[b2], in_=Xin_d[t * 128:(t + 1) * 128, :]),
                      reads=[B_in], writes=[B_xr[b2]])
                P.dma("act", lambda e, t=t, b2=b2: e.dma_start(out=tl[b2], in_=TT_d[t]), reads=[B_TT], writes=[B_tl[b2]])
                for c in range(2):
                    pbk = 2 * b2 + c
                    for ch in range(16):
                        P.pe(lambda e, ch=ch, c=c, b2=b2, pbk=pbk: e.matmul(
                            bank(pbk), tl[b2][:, ch * 128:(ch + 1) * 128], w2[:, ch, c * 512:(c + 1) * 512],
                            start=(ch == 0), stop=(ch == 15)), reads=[B_tl[b2], B_w2], writes=[PSB[pbk]])
                    P.dve(lambda e, c=c, b2=b2, pbk=pbk: e.tensor_tensor(
                        out=zt[b2][:, c * 512:(c + 1) * 512], in0=bank(pbk), in1=modbc[:, 2, c * 512:(c + 1) * 512], op=ALU.mult),
                        reads=[PSB[pbk], B_mod], writes=[B_zt[b2]])
                P.dve(lambda e, b2=b2: e.scalar_tensor_tensor(out=zt[b2], in0=xr[b2], scalar=ALPHA, in1=zt[b2],
                                                              op0=ALU.mult, op1=ALU.add),
                      reads=[B_xr[b2], B_zt[b2]], writes=[B_zt[b2]])
                layer_norm_store(zt[b2], B_zt[b2], 0, Xout_d[t * 128:(t + 1) * 128, :], B_out, tmp, B_tmp, s4[b2], B_s4[b2])
            P.barrier()

        def phase_moe(l, Xin_d, B_in, Xout_d, B_out):
            AR.off = persist_mark
            B_XS, B_YS, B_zf = Buf(), Buf(), Buf()
            dest_all = AR.alloc([32, 8], U32)
            w_all = AR.alloc([32, 8], F32)
            widx = AR.alloc([512], U32)
            B_dest, B_wall, B_widx = Buf(), Buf(), Buf()
            m_small = AR.off
            m_z = AR.off
            zt0 = AR.alloc([8192], BF16)
            AR.off = m_z
            wn_all = AR.alloc([32, 256], F32)
            B_z0 = Buf()
            P.pool(lambda e: e.memset(zt0, 0.0), writes=[B_z0])
            xsv = XS_d.rearrange("(n p r) d -> n p (r d)", p=128, r=8)
            for n in range(64):
                P.dma("act", lambda e, n=n: e.dma_start(out=xsv[n], in_=zt0), reads=[B_z0], writes=[B_zf])
            hball = AR.alloc([32, D], BF16)
            M_all = AR.alloc([32, 256], BF16)
            e8_all = AR.alloc([32, 8], F32)
            B_hball, B_Mall, B_wn, B_e8 = Buf(), Buf(), B_z0, Buf()
            w_r = AR.alloc([8, 256], BF16)
            rbias = AR.alloc([256], F32)
            B_wr = Buf()
            P.dma("pool", lambda e: e.dma_start(out=w_r, in_=router_w_in[l].rearrange("(k p) f -> p k f", p=128)), writes=[B_wr])
            P.dma("sp", lambda e: e.dma_start(out=rbias, in_=rbias_in[l]), writes=[B_wr])
            xt = [AR.alloc([D], F32)] * 2
            hT = AR.alloc([8, 128], BF16)
            sc = AR.alloc([256], F32)
            ch = AR.alloc([256], F32)
            mk = AR.alloc([256], F32)
            Mf = AR.alloc([256], F32)
            wt = AR.alloc([256], F32)
            m8 = AR.alloc([8, 8], F32)
            gsc = AR.alloc([8], F32)
            gs8 = AR.alloc([8], F32)
            gmk = AR.alloc([8], F32)
            t8 = AR.alloc([8], F32)
            i8 = AR.alloc([8], U32)
            ss = AR.alloc([2], F32)
            B_xt = [Buf()] * 2
            B_hT, B_r = Buf(), Buf()
            for t in range(32):
                b2 = t % 2
                P.dma("sp", lambda e, t=t, b2=b2: e.dma_start(out=xt[b2], in_=Xin_d[t * 128:(t + 1) * 128, :]),
                      reads=[B_in], writes=[B_xt[b2]])
                make_hT(xt[b2], B_xt[b2], modbc[:, 4, :], modbc[:, 3, :], B_mod, hball[:, t, :], B_hball, b2, hT, B_hT)
                for k in range(8):
                    P.pe(lambda e, k=k, b2=b2: e.matmul(bank(2 + b2)[:, 0:256], hT[:, k, :], w_r[:, k, :], start=(k == 0), stop=(k == 7)),
                         reads=[B_hT, B_wr], writes=[PSB[2 + b2]])
                P.act(lambda e, b2=b2: e.activation(out=sc, in_=bank(2 + b2)[:, 0:256], func=AF.Sigmoid), reads=[PSB[2 + b2]], writes=[B_r])
                P.dve(lambda e: e.tensor_tensor(out=ch, in0=sc, in1=rbias, op=ALU.add), reads=[B_r, B_wr], writes=[B_r])
                for g in range(8):
                    P.dve(lambda e, g=g: e.max(out=m8[:, g, :], in_=ch[:, g * 32:(g + 1) * 32]), reads=[B_r], writes=[B_r])
                P.dve(lambda e: e.tensor_tensor(out=gsc, in0=m8[:, :, 0], in1=m8[:, :, 1], op=ALU.add), reads=[B_r], writes=[B_r])
                P.dve(lambda e: e.max(out=gs8, in_=gsc), reads=[B_r], writes=[B_r])
                P.dve(lambda e: e.tensor_scalar(out=gmk, in0=gsc, scalar1=gs8[:, 3:4], scalar2=None, op0=ALU.is_ge), reads=[B_r], writes=[B_r])
                P.dve(lambda e: e.tensor_scalar(out=gmk, in0=gmk, scalar1=1e30, scalar2=-1e30, op0=ALU.mult, op1=ALU.add), reads=[B_r], writes=[B_r])
                P.dve(lambda e: e.tensor_tensor(out=mk.rearrange("p (g c) -> p g c", g=8), in0=ch.rearrange("p (g c) -> p g c", g=8),
                                                in1=gmk.unsqueeze(2).to_broadcast([128, 8, 32]), op=ALU.add), reads=[B_r], writes=[B_r])
                P.dve(lambda e: e.max(out=t8, in_=mk), reads=[B_r], writes=[B_r])
                P.dve(lambda e: e.tensor_scalar(out=Mf, in0=mk, scalar1=t8[:, 7:8], scalar2=None, op0=ALU.is_ge), reads=[B_r], writes=[B_r])
                P.dve(lambda e, t=t: e.tensor_copy(out=M_all[:, t, :], in_=Mf), reads=[B_r], writes=[B_Mall])
                P.dve(lambda e: e.scalar_tensor_tensor(out=wt, in0=sc, scalar=1.0, in1=Mf, op0=ALU.mult, op1=ALU.mult, accum_out=ss[:, 0:1]),
                      reads=[B_r], writes=[B_r])
                P.dve(lambda e: e.reciprocal(out=ss[:, 1:2], in_=ss[:, 0:1]), reads=[B_r], writes=[B_r])
                P.dve(lambda e, t=t: e.tensor_scalar(out=wn_all[:, t, :], in0=wt, scalar1=ss[:, 1:2], scalar2=2.5, op0=ALU.mult, op1=ALU.mult),
                      reads=[B_r], writes=[B_wn])
                P.dve(lambda e: e.max_index(out=i8, in_max=t8, in_values=mk), reads=[B_r], writes=[B_r])
                P.dve(lambda e, t=t: e.tensor_scalar_add(out=e8_all[:, t, :], in0=i8, scalar1=0.0), reads=[B_r], writes=[B_e8])
                P.pe(lambda e, t=t: e.matmul(bank(7)[:, 0:256], ones_b, M_all[:, t, :], start=(t == 0), stop=(t == 31)),
                     reads=[B_Mall, B_cst], writes=[PSB[7]])
            if DBG_M < 2:
                P.barrier()
                return
            cnt = AR.alloc([256], F32)
            ci = AR.alloc([256], I32)
            pad = AR.alloc([256], F32)
            cs_a = AR.alloc([256], F32)
            cs_b = AR.alloc([256], F32)
            pstart = AR.alloc([256], F32)
            pcol = AR.alloc([2], F32)
            junk = AR.alloc([256], F32)
            Ab = [AR.alloc([512], BF16) for _ in range(2)]
            bef = AR.alloc([512], F32)
            B_l = Buf()
            P.dve(lambda e: e.tensor_scalar_add(out=ci, in0=bank(7)[:, 0:256], scalar1=127.0), reads=[PSB[7]], writes=[B_l])
            P.dve(lambda e: e.tensor_single_scalar(out=ci, in_=ci, scalar=7, op=ALU.arith_shift_right), reads=[B_l], writes=[B_l])
            P.dve(lambda e: e.tensor_single_scalar(out=ci, in_=ci, scalar=7, op=ALU.logical_shift_left), reads=[B_l], writes=[B_l])
            P.dve(lambda e: e.tensor_scalar_add(out=pad, in0=ci, scalar1=0.0), reads=[B_l], writes=[B_l])
            P.dve(lambda e: e.tensor_copy(out=cs_a, in_=pad), reads=[B_l], writes=[B_l])
            cur, nxt = cs_a, cs_b
            sft = 1
            while sft < 256:
                P.dve(lambda e, cur=cur, nxt=nxt, sft=sft: e.tensor_copy(out=nxt[:, 0:sft], in_=cur[:, 0:sft]), reads=[B_l], writes=[B_l])
                P.dve(lambda e, cur=cur, nxt=nxt, sft=sft: e.tensor_tensor(out=nxt[:, sft:256], in0=cur[:, sft:256], in1=cur[:, 0:256 - sft], op=ALU.add),
                      reads=[B_l], writes=[B_l])
                cur, nxt = nxt, cur
                sft *= 2
            pends = cur
            P.dve(lambda e: e.tensor_tensor(out=pstart, in0=pends, in1=pad, op=ALU.subtract), reads=[B_l], writes=[B_l])
            for c in range(2):
                P.dve(lambda e, c=c: e.scalar_tensor_tensor(out=junk[:, 0:128], in0=pends[:, c * 128:(c + 1) * 128], scalar=1.0, in1=ident_f,
                                                            op0=ALU.mult, op1=ALU.mult, accum_out=pcol[:, c:c + 1]),
                      reads=[B_l, B_cst], writes=[B_l])
                P.dve(lambda e, c=c: e.tensor_scalar(out=Ab[c], in0=iota_blk, scalar1=pcol[:, c:c + 1], scalar2=None, op0=ALU.is_ge),
                      reads=[B_l, B_cst], writes=[B_l])
                P.pe(lambda e, c=c: e.matmul(bank(6), ones_b, Ab[c], start=(c == 0), stop=(c == 1)), reads=[B_l, B_cst], writes=[PSB[6]])
            P.dve(lambda e: e.tensor_scalar_min(out=bef, in0=bank(6), scalar1=255.0), reads=[PSB[6]], writes=[B_l])
            P.dve(lambda e: e.tensor_scalar(out=bef, in0=bef, scalar1=128.0, scalar2=iota_p, op0=ALU.mult, op1=ALU.add),
                  reads=[B_l, B_cst], writes=[B_l])
            P.dve(lambda e: e.tensor_scalar_add(out=widx, in0=bef, scalar1=float(l * NE * 128)), reads=[B_l], writes=[B_widx])
            if DBG_M < 3:
                P.barrier()
                return
            dm = AR.alloc([256], F32)
            d8 = AR.alloc([8], F32)
            B_dm = Buf()
            for t in range(32):
                b2 = t % 2
                P.pe(lambda e, t=t, b2=b2: e.matmul(bank(b2)[:, 0:256], ustr_b, M_all[:, t, :], start=True, stop=(t == 0)),
                     reads=[B_Mall, B_cst], writes=[PSB[b2]])
                for j in range(t):
                    P.pe(lambda e, j=j, t=t, b2=b2: e.matmul(bank(b2)[:, 0:256], ones_b, M_all[:, j, :], start=False, stop=(j == t - 1)),
                         reads=[B_Mall, B_cst], writes=[PSB[b2]])
                P.dve(lambda e, b2=b2: e.tensor_tensor(out=dm, in0=bank(b2)[:, 0:256], in1=pstart, op=ALU.add), reads=[PSB[b2], B_l], writes=[B_dm])
                for j in range(8):
                    P.dve(lambda e, t=t, j=j: e.scalar_tensor_tensor(out=junk, in0=iota_e, scalar=e8_all[:, t, j:j + 1], in1=dm,
                                                                     op0=ALU.is_equal, op1=ALU.mult, accum_out=d8[:, j:j + 1]),
                          reads=[B_e8, B_dm, B_cst], writes=[B_dm])
                    P.dve(lambda e, t=t, j=j: e.scalar_tensor_tensor(out=junk, in0=iota_e, scalar=e8_all[:, t, j:j + 1], in1=wn_all[:, t, :],
                                                                     op0=ALU.is_equal, op1=ALU.mult, accum_out=w_all[:, t, j:j + 1]),
                          reads=[B_e8, B_wn, B_cst], writes=[B_wall])
                P.dve(lambda e, t=t: e.tensor_scalar_add(out=dest_all[:, t, :], in0=d8, scalar1=0.0), reads=[B_dm], writes=[B_dest])
                if DBG_M == 3:
                    dbg = AR.alloc([24], F32) if t == 0 else dbg
                    B_dbg = Buf() if t == 0 else B_dbg
                    P.dve(lambda e, dbg=dbg: e.tensor_copy(out=dbg[:, 0:8], in_=d8), reads=[B_dm], writes=[B_dbg])
                    P.dve(lambda e, t=t, dbg=dbg: e.tensor_copy(out=dbg[:, 8:16], in_=w_all[:, t, :]), reads=[B_wall], writes=[B_dbg])
                    P.dve(lambda e, t=t, dbg=dbg: e.tensor_copy(out=dbg[:, 16:24], in_=e8_all[:, t, :]), reads=[B_e8], writes=[B_dbg])
                    P.dma("sp", lambda e, t=t, dbg=dbg: e.dma_start(out=out_d[t * 128:(t + 1) * 128, 0:24], in_=dbg), reads=[B_dbg], writes=[B_OUT])
                for j in range(8 if DBG_M >= 4 else 0):
                    P.dma("pool", lambda e, t=t, j=j: e.indirect_dma_start(
                        out=XS_d, out_offset=bass.IndirectOffsetOnAxis(ap=dest_all[:, t, j:j + 1], axis=0),
                        in_=hball[:, t, :], in_offset=None), reads=[B_dest, B_hball, B_zf], writes=[B_XS])
            P.barrier()
            if DBG_M < 5:
                return
            AR.off = m_small
            wg = [AR.alloc([2048], BF16) for _ in range(2)]
            wu = [AR.alloc([2048], BF16) for _ in range(2)]
            wd = [AR.alloc([2048], BF16) for _ in range(2)]
            xb = [AR.alloc([D], BF16) for _ in range(2)]
            xT = [AR.alloc([8, 128], BF16) for _ in range(2)]
            g32 = AR.alloc([256], F32)
            Gb = AR.alloc([256], BF16)
            GT = AR.alloc([2, 128], BF16)
            yb = [AR.alloc([D], BF16) for _ in range(2)]
            B_wgu, B_wd, B_xb, B_xT, B_yb = ([Buf(), Buf()] for _ in range(5))
            B_g32, B_G, B_GT = Buf(), Buf(), Buf()
            for b in range(NBLK):
                b2 = b % 2
                off = bass.IndirectOffsetOnAxis(ap=widx[:, b:b + 1], axis=0)
                P.dma("pool", lambda e, b2=b2, off=off: e.indirect_dma_start(out=wg[b2], out_offset=None, in_=exp_g_in, in_offset=off),
                      reads=[B_widx], writes=[B_wgu[b2]])
                P.dma("pool", lambda e, b2=b2, off=off: e.indirect_dma_start(out=wu[b2], out_offset=None, in_=exp_u_in, in_offset=off),
                      reads=[B_widx], writes=[B_wgu[b2]])
                P.dma("pool", lambda e, b2=b2, off=off: e.indirect_dma_start(out=wd[b2], out_offset=None, in_=exp_d_in, in_offset=off),
                      reads=[B_widx], writes=[B_wd[b2]])
                P.dma("sp", lambda e, b=b, b2=b2: e.dma_start(out=xb[b2], in_=XS_d[b * 128:(b + 1) * 128, :]), reads=[B_XS], writes=[B_xb[b2]])
                pt = bank_bf(b2)
                xv = xb[b2].rearrange("s (p k) -> s k p", k=8)
                for k in range(8):
                    P.pe(lambda e, k=k, pt=pt, xv=xv: e.transpose(out=pt[:, k * 128:(k + 1) * 128], in_=xv[:, k, :], identity=ident_b),
                         reads=[B_xb[b2], B_cst], writes=[PSB[b2]])
                P.act(lambda e, b2=b2, pt=pt: e.copy(out=xT[b2], in_=pt.rearrange("p (a b) -> p a b", a=8)), reads=[PSB[b2]], writes=[B_xT[b2]])
                for (wsrc, c0) in ((wg, 0), (wu, 256)):
                    for k in range(8):
                        P.pe(lambda e, k=k, b2=b2, wsrc=wsrc, c0=c0: e.matmul(bank(2 + b2)[:, c0:c0 + 256], xT[b2][:, k, :],
                                                                              wsrc[b2][:, k * 256:(k + 1) * 256], start=(k == 0), stop=(k == 7)),
                             reads=[B_xT[b2], B_wgu[b2]], writes=[PSB[2 + b2]])
                P.act(lambda e, b2=b2: e.activation(out=g32, in_=bank(2 + b2)[:, 0:256], func=AF.Silu), reads=[PSB[2 + b2]], writes=[B_g32])
                P.dve(lambda e, b2=b2: e.tensor_tensor(out=Gb, in0=bank(2 + b2)[:, 256:512], in1=g32, op=ALU.mult),
                      reads=[PSB[2 + b2], B_g32], writes=[B_G])
                gv = Gb.rearrange("s (p j) -> s j p", j=2)
                pg = bank_bf(4 + b2)
                for j in range(2):
                    P.pe(lambda e, j=j, pg=pg, gv=gv: e.transpose(out=pg[:, j * 128:(j + 1) * 128], in_=gv[:, j, :], identity=ident_b),
                         reads=[B_G, B_cst], writes=[PSB[4 + b2]])
                P.dve(lambda e, pg=pg: e.tensor_copy(out=GT, in_=pg[:, 0:256].rearrange("p (a b) -> p a b", a=2)), reads=[PSB[4 + b2]], writes=[B_GT])
                for c in range(2):
                    for j in range(2):
                        P.pe(lambda e, c=c, j=j, b2=b2: e.matmul(bank(6 + c), GT[:, j, :], wd[b2][:, j * D + c * 512:j * D + (c + 1) * 512],
                                                                 start=(j == 0), stop=(j == 1)), reads=[B_GT, B_wd[b2]], writes=[PSB[6 + c]])
                P.dve(lambda e, b2=b2: e.tensor_copy(out=yb[b2][:, 0:512], in_=bank(6)), reads=[PSB[6]], writes=[B_yb[b2]])
                P.act(lambda e, b2=b2: e.copy(out=yb[b2][:, 512:1024], in_=bank(7)), reads=[PSB[7]], writes=[B_yb[b2]])
                P.dma("act", lambda e, b=b, b2=b2: e.dma_start(out=YS_d[b * 128:(b + 1) * 128, :], in_=yb[b2]), reads=[B_yb[b2]], writes=[B_YS])
            P.barrier()
            if DBG_M < 6:
                return
            AR.off = m_small
            sgu_w = AR.alloc([8, 512], BF16)
            sdw = AR.alloc([2, D], BF16)
            B_sw = Buf()
            P.dma("pool", lambda e: e.dma_start(out=sgu_w[:, :, 0:256], in_=sh_g_in[l].rearrange("(k p) f -> p k f", p=128)), writes=[B_sw])
            P.dma("pool", lambda e: e.dma_start(out=sgu_w[:, :, 256:512], in_=sh_u_in[l].rearrange("(k p) f -> p k f", p=128)), writes=[B_sw])
            P.dma("pool", lambda e: e.dma_start(out=sdw, in_=sh_d_in[l].rearrange("(j p) d -> p j d", p=128)), writes=[B_sw])
            yg = [AR.alloc([D], BF16) for _ in range(8)]
            B_yg = [Buf() for _ in range(8)]
            acc = AR.alloc([D], F32)
            xt2 = AR.alloc([D], F32)
            xr = AR.alloc([D], F32)
            hb2 = AR.alloc([D], BF16)
            hT2 = AR.alloc([8, 128], BF16)
            zt = AR.alloc([D], F32)
            tmp = AR.alloc([D], F32)
            s4 = AR.alloc([4], F32)
            B_acc, B_xt2, B_xr, B_hb2, B_hT2, B_zt, B_tmp, B_s4 = (Buf() for _ in range(8))
            for t in range(32):
                for j in range(8):
                    P.dma("pool", lambda e, t=t, j=j: e.indirect_dma_start(
                        out=yg[j], out_offset=None, in_=YS_d, in_offset=bass.IndirectOffsetOnAxis(ap=dest_all[:, t, j:j + 1], axis=0)),
                        reads=[B_YS, B_dest], writes=[B_yg[j]])
                P.dve(lambda e, t=t: e.tensor_scalar_mul(out=acc, in0=yg[0], scalar1=w_all[:, t, 0:1]), reads=[B_yg[0], B_wall], writes=[B_acc])
                for j in range(1, 8):
                    P.dve(lambda e, t=t, j=j: e.scalar_tensor_tensor(out=acc, in0=yg[j], scalar=w_all[:, t, j:j + 1], in1=acc,
                                                                     op0=ALU.mult, op1=ALU.add), reads=[B_yg[j], B_wall, B_acc], writes=[B_acc])
                P.dma("sp", lambda e, t=t: e.dma_start(out=xt2, in_=Xin_d[t * 128:(t + 1) * 128, :]), reads=[B_in], writes=[B_xt2])
                P.dma("sp", lambda e, t=t: e.dma_start(out=xr, in_=Xin_d[t * 128:(t + 1) * 128, :]), reads=[B_in], writes=[B_xr])
                make_hT(xt2, B_xt2, modbc[:, 4, :], modbc[:, 3, :], B_mod, hb2, B_hb2, 0, hT2, B_hT2)
                for k in range(8):
                    P.pe(lambda e, k=k: e.matmul(bank(2), hT2[:, k, :], sgu_w[:, k, :], start=(k == 0), stop=(k == 7)),
                         reads=[B_hT2, B_sw], writes=[PSB[2]])
                P.act(lambda e: e.activation(out=g32, in_=bank(2)[:, 0:256], func=AF.Silu), reads=[PSB[2]], writes=[B_g32])
                P.dve(lambda e: e.tensor_tensor(out=Gb, in0=bank(2)[:, 256:512], in1=g32, op=ALU.mult), reads=[PSB[2], B_g32], writes=[B_G])
                pg = bank_bf(4)
                for j in range(2):
                    P.pe(lambda e, j=j, pg=pg: e.transpose(out=pg[:, j * 128:(j + 1) * 128], in_=Gb[:, j * 128:(j + 1) * 128], identity=ident_b),
                         reads=[B_G, B_cst], writes=[PSB[4]])
                P.dve(lambda e, pg=pg: e.tensor_copy(out=GT, in_=pg[:, 0:256].rearrange("p (a b) -> p a b", a=2)), reads=[PSB[4]], writes=[B_GT])
                for c in range(2):
                    for j in range(2):
                        P.pe(lambda e, c=c, j=j: e.matmul(bank(6 + c), GT[:, j, :], sdw[:, j, c * 512:(c + 1) * 512], start=(j == 0), stop=(j == 1)),
                             reads=[B_GT, B_sw], writes=[PSB[6 + c]])
                    P.dve(lambda e, c=c: e.tensor_tensor(out=zt[:, c * 512:(c + 1) * 512], in0=bank(6 + c), in1=acc[:, c * 512:(c + 1) * 512], op=ALU.add),
                          reads=[PSB[6 + c], B_acc], writes=[B_zt])
                P.dve(lambda e: e.tensor_tensor(out=zt, in0=zt, in1=modbc[:, 5, :], op=ALU.mult), reads=[B_zt, B_mod], writes=[B_zt])
                P.dve(lambda e: e.scalar_tensor_tensor(out=zt, in0=xr, scalar=ALPHA, in1=zt, op0=ALU.mult, op1=ALU.add),
                      reads=[B_xr, B_zt], writes=[B_zt])
                layer_norm_store(zt, B_zt, 1, Xout_d[t * 128:(t + 1) * 128, :], B_out, tmp, B_tmp, s4, B_s4)
            P.barrier()

        if stop_after == "Btest":
            phase_mod(0, False)
            phase_moe(0, x_in, Buf(), out_d, B_OUT)
            P.emit(nc, st)
            return nc, P
        if stop_after == "Ctest":
            phase_mod(1, False)
            phase_sgu(x_in, Buf(), out_d, B_OUT)
            P.emit(nc, st)
            return nc, P
        phase_mod(0, True)
        if stop_after == "M":
            for ch in range(6):
                P.dma("sp", lambda e, ch=ch: e.dma_start(out=out_d[ch * 128:(ch + 1) * 128, :], in_=modbc[:, ch, :]),
                      reads=[B_mod], writes=[B_OUT])
            for ch in range(2):
                P.dma("sp", lambda e, ch=ch: e.dma_start(out=out_d[(6 + ch) * 128:(7 + ch) * 128, :], in_=modcc[:, ch, :]),
                      reads=[B_modc], writes=[B_OUT])
            P.emit(nc, st)
            return nc, P
        phase_attn()
        if stop_after == "D":
            phase_moe(0, X1_d, B_X1, X2_d, B_X2)
            phase_mod(1, False)
            phase_sgu(X2_d, B_X2, X3_d, B_X3)
            phase_moe(1, X3_d, B_X3, out_d, B_OUT)
            P.emit(nc, st)
            return nc, P
        if stop_after == "A1":
            P.emit(nc, st)
            return nc, P
        if stop_after == "A":
            AR.off = persist_mark
            cp = [AR.alloc([D], F32) for _ in range(2)]
            B_cp = [Buf(), Buf()]
            for t in range(32):
                b2 = t % 2
                P.dma("sp", lambda e, t=t, b2=b2: e.dma_start(out=cp[b2], in_=X1_d[t * 128:(t + 1) * 128, :]),
                      reads=[B_X1], writes=[B_cp[b2]])
                P.dma("sp", lambda e, t=t, b2=b2: e.dma_start(out=out_d[t * 128:(t + 1) * 128, :], in_=cp[b2]),
                      reads=[B_cp[b2]], writes=[B_OUT])
        P.emit(nc, st)
    return nc, P


def _consts():
    cst = np.zeros((128, 5, 128), np.float32)
    cst[:, 0, :] = np.eye(128, dtype=np.float32)
    cst[:, 1, :] = 1.0
    cst[:, 2, :] = np.triu(np.ones((128, 128), np.float32), 1)
    perm = np.zeros((128, 128), np.float32)
    for dst in range(128):
        i = dst % 32
        src = dst + 16 if i < 16 else dst - 16
        perm[src, dst] = 1.0
    cst[:, 3, :] = perm
    cst[:, 4, :] = 1.0 / 128.0
    iota = np.zeros((128, 256 + 512 + 1), np.float32)
    iota[:, 0:256] = np.arange(256, dtype=np.float32)[None, :]
    iota[:, 256:768] = (np.arange(512, dtype=np.float32) * 128.0)[None, :]
    iota[:, 768] = np.arange(128, dtype=np.float32)
    return cst, iota


def _rope_tables():
    n = np.arange(NSEQ)
    row = (n // 64).astype(np.float32)
    col = (n % 64).astype(np.float32)
    inv = (10000.0 ** (-np.arange(0, 32, 2, dtype=np.float32) / np.float32(32))).astype(np.float32)
    cosT = np.zeros((128, NSEQ), np.float32)
    sinT = np.zeros((128, NSEQ), np.float32)
    for p in range(128):
        i = p % 64
        pos = row if i < 32 else col
        jj = i % 32
        ang = (pos * inv[jj % 16]).astype(np.float32)
        cosT[p] = np.cos(ang)
        sinT[p] = np.sin(ang) * (-1.0 if jj < 16 else 1.0)
    return cosT, sinT


def _bc(v):
    return np.ascontiguousarray(np.broadcast_to(np.asarray(v, np.float32).reshape(1, -1), (128, v.size)))


_CACHE = {}


def make_in_maps(inputs, cores, stop_after):
    cst, iota = _consts()
    cosT, sinT = _rope_tables()
    f = lambda k: np.asarray(inputs[k], np.float32)
    x, c, ctx, c_ctx = f("x"), f("c"), f("ctx"), f("c_ctx")
    w_mod, b_mod, ln_g, ln_b = f("w_mod"), f("b_mod"), f("ln_g"), f("ln_b")
    b_mod_bc = np.ascontiguousarray(np.broadcast_to(b_mod[:, None, :], (2, 128, 6 * D)))
    ln_bc = np.zeros((2, 2, 2, 128, D), np.float32)
    for l in range(2):
        for a in range(2):
            ln_bc[l, a, 0] = ln_g[l, a][None, :]
            ln_bc[l, a, 1] = ln_b[l, a][None, :]
    maps = []
    for core in cores:
        b, half = core // 2, core % 2
        order = np.concatenate([np.arange(half * NTOK, (half + 1) * NTOK), np.arange((1 - half) * NTOK, (2 - half) * NTOK)])
        m = {
            "x": np.ascontiguousarray(x[b][order]),
            "ctx": np.ascontiguousarray(ctx[b]),
            "c_bc": _bc(c[b]), "cc_bc": _bc(c_ctx),
            "w_mod": w_mod, "b_mod_bc": b_mod_bc, "ln_bc": ln_bc,
            "attn_w_in": f("attn_w_in")[0], "attn_w_out": f("attn_w_out")[0],
            "lam_bc": _bc(f("attn_lambda")[0].reshape(-1)),
            "subln_col": np.ascontiguousarray(f("attn_subln_g")[0].reshape(128, 1)),
            "cosT": np.ascontiguousarray(cosT[:, order]), "sinT": np.ascontiguousarray(sinT[:, order]),
            "cst": cst, "iota": iota,
            "router_w": f("router_w"), "rbias_bc": np.ascontiguousarray(np.broadcast_to(f("router_bias")[:, None, :], (2, 128, NE))),
            "exp_w_gate": f("exp_w_gate").reshape(2 * NE * 128, 2048), "exp_w_up": f("exp_w_up").reshape(2 * NE * 128, 2048),
            "exp_w_down": f("exp_w_down").reshape(2 * NE * 128, 2048),
            "sh_w_gate": f("sh_w_gate"), "sh_w_up": f("sh_w_up"), "sh_w_down": f("sh_w_down"),
            "sgu_w_in": f("sgu_w_in")[0], "sgu_w_out": f("sgu_w_out")[0],
            "sgu_ngb": np.stack([_bc(f("sgu_norm_g")[0]), _bc(f("sgu_norm_b")[0])]),
            "sgu_binv": _bc(f("sgu_b_in")[0][2048:]),
            "sgu_binu": np.ascontiguousarray(f("sgu_b_in")[0][:2048].reshape(16, 128).T),
            "sgu_bs16": _bc(np.repeat(f("sgu_b_s")[0], 2, axis=0).reshape(-1)),
            "sgu_wsT": np.ascontiguousarray(f("sgu_w_s")[0].transpose(2, 0, 1)),
        }
        maps.append(m)
    return maps


def kernel(**inputs):
    stop_after = "D"
    if "nc" not in _CACHE:
        _CACHE["nc"] = build(stop_after)[0]
    nc = _CACHE["nc"]
    cores = list(range(8))
    maps = make_in_maps(inputs, cores, stop_after)
    res = run_bass_kernel_spmd(nc, maps, core_ids=cores)
    out = np.zeros((4, NSEQ, D), np.float32)
    for core in cores:
        b, half = core // 2, core % 2
        out[b, half * NTOK:(half + 1) * NTOK] = res.results[core]["out"]
    return out
```

```python
import math
import os
DBG_G = int(os.environ.get('DBG_G', '17'))
DBG_J = int(os.environ.get('DBG_J', '16'))
DBG_V = int(os.environ.get('DBG_V', '1'))
DBG_R = int(os.environ.get('DBG_R', '1'))
DBG_DUMP = int(os.environ.get('DBG_DUMP', '1'))
DBG_W = int(os.environ.get('DBG_W', '1'))
DBG_K = int(os.environ.get('DBG_K', '9'))
DBG_X = int(os.environ.get('DBG_X', '0'))
DBG_M = int(os.environ.get('DBG_M', '9'))
from contextlib import ExitStack

import numpy as np
import concourse.bass as bass
import concourse.mybir as mybir
from concourse.bass_utils import run_bass_kernel_spmd

F32 = mybir.dt.float32
BF16 = mybir.dt.bfloat16
U32 = mybir.dt.uint32
I32 = mybir.dt.int32
ALU = mybir.AluOpType
AF = mybir.ActivationFunctionType
AX = mybir.AxisListType

COMPUTE = ("pe", "act", "dve", "pool")
QUEUES = ("act", "pool", "sp")
NSLOT = 20

D = 1024
NTOK = 4096
NSEQ = 8192
NCTX = 256
NKEY = NSEQ + NCTX
NE = 256
ALPHA = 4.0 ** 0.25
LN_EPS = 1e-5
NBLK = 512
NSLOTS = NBLK * 128


class Buf:
    __slots__ = ("name", "w", "r")

    def __init__(self, name=""):
        self.name = name
        self.w = []
        self.r = {}


class Ins:
    __slots__ = ("fn", "eng", "dma", "deps", "needed", "cnt", "slot", "qidx", "bar")

    def __init__(self, fn, eng, dma):
        self.fn = fn
        self.eng = eng
        self.dma = dma
        self.deps = []
        self.needed = False
        self.cnt = 0
        self.slot = 0
        self.qidx = 0
        self.bar = False


class Prog:
    def __init__(self):
        self.q = {e: [] for e in ("pe", "act", "dve", "pool", "sp")}
        self.ndma = {e: 0 for e in QUEUES}
        self.lastdma = {e: {} for e in QUEUES}
        self.n = 0

    def op(self, eng, fn, reads=(), writes=(), dma=False):
        ins = Ins(fn, eng, dma)
        deps = {}
        raw = set()
        for b in reads:
            for w in b.w:
                deps[id(w)] = w
                raw.add(id(w))
        for b in writes:
            for w in b.w:
                if dma and w.dma:
                    continue
                deps[id(w)] = w
            for r in b.r.values():
                deps[id(r)] = r
        for d in deps.values():
            if d is ins:
                continue
            if d.eng == eng and not d.dma and not dma:
                if eng == "pe" or id(d) not in raw:
                    continue
            ins.deps.append(d)
            d.needed = True
        if dma:
            ins.qidx = self.ndma[eng]
            self.ndma[eng] += 1
            self.lastdma[eng][ins.qidx % NSLOT] = ins
            key = (eng, "d", ins.qidx)
        else:
            key = eng
        for b in reads:
            b.r[key] = ins
        for b in writes:
            if dma and b.w and all(w.dma for w in b.w) and not b.r:
                b.w = b.w + [ins]
            else:
                b.w = [ins]
            b.r = {}
        self.q[eng].append(ins)
        self.n += 1
        return ins

    def pe(self, fn, reads=(), writes=()):
        return self.op("pe", fn, reads, writes)

    def act(self, fn, reads=(), writes=()):
        return self.op("act", fn, reads, writes)

    def dve(self, fn, reads=(), writes=()):
        return self.op("dve", fn, reads, writes)

    def pool(self, fn, reads=(), writes=()):
        return self.op(os.environ.get('POOL_ENG', 'pool'), fn, reads, writes)

    def dma(self, q, fn, reads=(), writes=()):
        return self.op(q, fn, reads, writes, dma=True)

    def barrier(self):
        deps = []
        for e in COMPUTE:
            for ins in reversed(self.q[e]):
                if not ins.dma and not ins.bar:
                    deps.append(ins)
                    ins.needed = True
                    break
        for e in QUEUES:
            deps.extend(self.lastdma[e].values())
        for e in self.q:
            ins = Ins(None, e, False)
            ins.bar = True
            ins.deps = [d for d in deps if not (d.eng == e and not d.dma)]
            self.q[e].append(ins)

    def emit(self, nc, stack):
        esem = {e: stack.enter_context(nc.semaphore("s_" + e)) for e in COMPUTE}
        dsem = {e: [stack.enter_context(nc.semaphore("d_%s_%d" % (e, i))) for i in range(NSLOT)]
                for e in QUEUES}
        for e in COMPUTE:
            c = 0
            for ins in self.q[e]:
                if ins.dma or ins.bar:
                    continue
                if ins.needed:
                    c += 1
                    ins.cnt = c
        for e in QUEUES:
            for ins in self.q[e]:
                if ins.dma:
                    ins.slot = ins.qidx % NSLOT
                    ins.cnt = (ins.qidx // NSLOT + 1) * 16
        block = stack.enter_context(nc.Block())
        prog = self

        def run(ename, eng):
            seen = {}

            def wait(sem, val):
                k = sem.num
                if seen.get(k, -1) >= val:
                    return
                seen[k] = val
                eng.wait_ge(sem, val)

            for ins in prog.q[ename]:
                for d in ins.deps:
                    if d.dma:
                        wait(dsem[d.eng][d.slot], d.cnt)
                    else:
                        wait(esem[d.eng], d.cnt)
                if ins.bar:
                    continue
                if ins.dma:
                    if ins.cnt > 16:
                        wait(dsem[ename][ins.slot], ins.cnt - 16)
                    ins.fn(eng).then_inc(dsem[ename][ins.slot], 16)
                else:
                    r = ins.fn(eng)
                    if ins.needed:
                        r.then_inc(esem[ename], 1)
            if ename in prog.lastdma:
                for sl, ins in prog.lastdma[ename].items():
                    wait(dsem[ename][sl], ins.cnt)

        @block.tensor
        def _(eng):
            run("pe", eng)

        @block.scalar
        def _(eng):
            run("act", eng)

        @block.vector
        def _(eng):
            run("dve", eng)

        @block.gpsimd
        def _(eng):
            run("pool", eng)

        @block.sync
        def _(eng):
            run("sp", eng)


class Arena:
    def __init__(self, nc, st, nbytes):
        self.t = st.enter_context(nc.sbuf_tensor("arena", [128, nbytes // 4], F32))
        self.off = 0
        self.cap = nbytes

    def alloc(self, shape, dtype):
        sz = 2 if dtype == BF16 else 4
        nelem = int(np.prod(shape))
        nb = (nelem * sz + 63) // 64 * 64
        a = self.off
        self.off += nb
        assert self.off <= self.cap, ("arena overflow", self.off, self.cap)
        v = self.t[:, a // 4:(a + nb) // 4]
        if dtype != F32:
            v = v.bitcast(dtype)
        v = v[:, :nelem]
        if len(shape) == 2:
            v = v.rearrange("p (a b) -> p a b", a=shape[0])
        elif len(shape) == 3:
            v = v.rearrange("p (a b c) -> p a b c", a=shape[0], b=shape[1])
        return v


def build(stop_after="D"):
    nc = bass.Bass("TRN2", target_bir_lowering=False)
    P = Prog()

    def din(name, shape, dtype=F32):
        return nc.dram_tensor(name, list(shape), dtype, kind="ExternalInput").ap()

    def dscr(name, shape, dtype):
        return nc.dram_tensor(name, list(shape), dtype, kind="Internal").ap()

    x_in = din("x", [NSEQ, D])
    ctx_in = din("ctx", [NCTX, D])
    c_bc_in = din("c_bc", [128, D])
    cc_bc_in = din("cc_bc", [128, D])
    w_mod_in = din("w_mod", [2, D, 6 * D])
    b_mod_in = din("b_mod_bc", [2, 128, 6 * D])
    ln_in = din("ln_bc", [2, 2, 2, 128, D])
    w_in_in = din("attn_w_in", [D, 3 * D])
    w_out_in = din("attn_w_out", [D, D])
    lam_in = din("lam_bc", [128, 4 * 64])
    subln_in = din("subln_col", [128, 1])
    cos_in = din("cosT", [128, NSEQ])
    sin_in = din("sinT", [128, NSEQ])
    cst_in = din("cst", [128, 5, 128])
    iota_in = din("iota", [128, 256 + 512 + 1])
    router_w_in = din("router_w", [2, D, NE])
    rbias_in = din("rbias_bc", [2, 128, NE])
    exp_g_in = din("exp_w_gate", [2 * NE * 128, 2048])
    exp_u_in = din("exp_w_up", [2 * NE * 128, 2048])
    exp_d_in = din("exp_w_down", [2 * NE * 128, 2048])
    sh_g_in = din("sh_w_gate", [2, D, 256])
    sh_u_in = din("sh_w_up", [2, D, 256])
    sh_d_in = din("sh_w_down", [2, 256, D])
    sgu_w_in_in = din("sgu_w_in", [D, 4096])
    sgu_w_out_in = din("sgu_w_out", [2048, D])
    sgu_ngb_in = din("sgu_ngb", [2, 128, 2048])
    sgu_binv_in = din("sgu_binv", [128, 2048])
    sgu_binu_in = din("sgu_binu", [128, 16])
    sgu_bs16_in = din("sgu_bs16", [128, 2048])
    sgu_wsT_in = din("sgu_wsT", [128, 8, 128])
    out_d = nc.dram_tensor("out", [NTOK, D], F32, kind="ExternalOutput").ap()

    KT_d = dscr("KT_d", [8, 128, NKEY], BF16)
    V_d = dscr("V_d", [NKEY, D], BF16)
    QT_d = dscr("QT_d", [8, 128, NTOK], BF16)
    X1_d = dscr("X1_d", [NTOK, D], F32)
    X2_d = dscr("X2_d", [NTOK, D], F32)
    X3_d = dscr("X3_d", [NTOK, D], F32)
    XS_d = dscr("XS_d", [NSLOTS, D], BF16)
    YS_d = dscr("YS_d", [NSLOTS, D], BF16)
    TT_d = dscr("TT_d", [32, 128, 2048], BF16)
    B_TT = Buf("TT")
    B_KT, B_V, B_QT = Buf("KT_d"), Buf("V_d"), Buf("QT_d")
    B_X1, B_X2, B_X3, B_OUT = Buf("X1"), Buf("X2"), Buf("X3"), Buf("OUT")

    with ExitStack() as st:
        AR = Arena(nc, st, 204 * 1024)
        psum = st.enter_context(nc.psum_tensor("psum", [128, 8, 512], F32))
        PSB = [Buf("ps%d" % i) for i in range(8)]

        def bank(i):
            return psum[:, i, :]

        def bank_bf(i):
            return psum[:, i, :].bitcast(BF16)

        cst_f = AR.alloc([5, 128], F32)
        cst_b = AR.alloc([5, 128], BF16)
        iota = AR.alloc([256 + 512 + 1], F32)
        B_cst = Buf("cst")
        P.dma("sp", lambda e: e.dma_start(out=cst_f, in_=cst_in), writes=[B_cst])
        P.dma("pool", lambda e: e.dma_start(out=cst_b, in_=cst_in), writes=[B_cst])
        P.dma("sp", lambda e: e.dma_start(out=iota, in_=iota_in), writes=[B_cst])
        ident_f, ones_f, onesd_f = cst_f[:, 0, :], cst_f[:, 1, :], cst_f[:, 4, :]
        ident_b, ones_b, ustr_b, perm_b = cst_b[:, 0, :], cst_b[:, 1, :], cst_b[:, 2, :], cst_b[:, 3, :]
        iota_e = iota[:, 0:256]
        iota_blk = iota[:, 256:768]
        iota_p = iota[:, 768:769]

        modbc = AR.alloc([6, D], F32)
        modcc = AR.alloc([2, D], F32)
        lnbc = AR.alloc([4, D], F32)
        B_mod, B_modc, B_ln = Buf("mod"), Buf("modc"), Buf("ln")
        persist_mark = AR.off

        def phase_mod(l, with_ctx):
            AR.off = persist_mark
            cb = AR.alloc([D], F32)
            sb = AR.alloc([D], F32)
            sT = AR.alloc([8, 128], F32)
            bmod = AR.alloc([6 * D], F32)
            wblk = [AR.alloc([8, 512], F32) for _ in range(2)]
            B_cb, B_sb, B_sT, B_bm = Buf(), Buf(), Buf(), Buf()
            B_w = [Buf(), Buf()]
            P.dma("sp", lambda e: e.dma_start(out=bmod, in_=b_mod_in[l]), writes=[B_bm])
            P.dma("act", lambda e: e.dma_start(out=lnbc, in_=ln_in[l].rearrange("a b p d -> p (a b) d")),
                  writes=[B_ln])
            srcs = [(c_bc_in, modbc, B_mod, 12)]
            if with_ctx:
                srcs.append((cc_bc_in, modcc, B_modc, 4))
            blk = 0
            for (src, dst, B_dst, nblk) in srcs:
                P.dma("sp", lambda e, src=src: e.dma_start(out=cb, in_=src), writes=[B_cb])
                P.act(lambda e: e.activation(out=sb, in_=cb, func=AF.Silu), reads=[B_cb], writes=[B_sb])
                for k in range(8):
                    P.pe(lambda e, k=k: e.transpose(out=bank(0)[:, k * 128:(k + 1) * 128] if k < 4 else
                                                     bank(1)[:, (k - 4) * 128:(k - 3) * 128],
                                                     in_=sb[:, k * 128:(k + 1) * 128], identity=ident_f),
                         reads=[B_sb, B_cst], writes=[PSB[k // 4]])
                P.dve(lambda e: e.tensor_copy(out=sT[:, 0:4, :], in_=bank(0).rearrange("p (a b) -> p a b", a=4)),
                      reads=[PSB[0]], writes=[B_sT])
                P.dve(lambda e: e.tensor_copy(out=sT[:, 4:8, :], in_=bank(1).rearrange("p (a b) -> p a b", a=4)),
                      reads=[PSB[1]], writes=[B_sT])
                dflat = dst.rearrange("p a b -> p (a b)")
                for n in range(nblk):
                    wb, Bw = wblk[blk % 2], B_w[blk % 2]
                    pb = 2 + blk % 2
                    blk += 1
                    P.dma("sp", lambda e, n=n, wb=wb: e.dma_start(
                        out=wb, in_=w_mod_in[l][:, n * 512:(n + 1) * 512].rearrange("(k p) n -> p k n", p=128)),
                        writes=[Bw])
                    for k in range(8):
                        P.pe(lambda e, k=k, wb=wb, pb=pb: e.matmul(bank(pb), sT[:, k, :], wb[:, k, :],
                                                                   start=(k == 0), stop=(k == 7)),
                             reads=[B_sT, Bw], writes=[PSB[pb]])
                    P.dve(lambda e, n=n, pb=pb, dflat=dflat: e.tensor_tensor(
                        out=dflat[:, n * 512:(n + 1) * 512], in0=bank(pb), in1=bmod[:, n * 512:(n + 1) * 512],
                        op=ALU.add), reads=[PSB[pb], B_bm], writes=[B_dst])
                for ch in ((1, 4) if nblk == 12 else (1,)):
                    P.dve(lambda e, ch=ch, dst=dst: e.tensor_scalar_add(out=dst[:, ch, :], in0=dst[:, ch, :],
                                                                       scalar1=1.0),
                          reads=[B_dst], writes=[B_dst])
            P.barrier()

        def layer_norm_store(zt, B_z, gi, out_ap, B_out, tmp, B_tmp, st4, B_st, q="sp"):
            mean, var, rstd, nmean = st4[:, 0:1], st4[:, 1:2], st4[:, 2:3], st4[:, 3:4]
            P.dve(lambda e: e.tensor_reduce(out=mean, in_=zt, axis=AX.X, op=ALU.add), reads=[B_z], writes=[B_st])
            P.dve(lambda e: e.tensor_scalar_mul(out=nmean, in0=mean, scalar1=-1.0 / D), reads=[B_st], writes=[B_st])
            P.act(lambda e: e.activation(out=zt, in_=zt, func=AF.Identity, bias=nmean, scale=1.0),
                  reads=[B_z, B_st], writes=[B_z])
            P.act(lambda e: e.activation(out=tmp, in_=zt, func=AF.Square, accum_out=var),
                  reads=[B_z], writes=[B_tmp, B_st])
            P.act(lambda e: e.activation(out=rstd, in_=var, func=AF.Sqrt, bias=eps_col, scale=1.0 / D),
                  reads=[B_st, B_cst2], writes=[B_st])
            P.dve(lambda e: e.reciprocal(out=rstd, in_=rstd), reads=[B_st], writes=[B_st])
            P.dve(lambda e: e.scalar_tensor_tensor(out=zt, in0=zt, scalar=rstd, in1=lnbc[:, 2 * gi, :],
                                                   op0=ALU.mult, op1=ALU.mult),
                  reads=[B_z, B_st, B_ln], writes=[B_z])
            P.pool(lambda e: e.tensor_tensor(out=zt, in0=zt, in1=lnbc[:, 2 * gi + 1, :], op=ALU.add),
                   reads=[B_z, B_ln], writes=[B_z])
            P.dma(q, lambda e: e.dma_start(out=out_ap, in_=zt), reads=[B_z], writes=[B_out])

        eps_col = AR.alloc([1], F32)
        B_cst2 = Buf()
        P.dve(lambda e: e.memset(eps_col, LN_EPS), writes=[B_cst2])
        persist_mark = AR.off

        def make_hT(xt, B_x, sc, sh, B_m, hb, B_hb, pb, hT_dst, B_hT):
            P.dve(lambda e: e.tensor_tensor(out=xt, in0=xt, in1=sc, op=ALU.mult), reads=[B_x, B_m], writes=[B_x])
            P.dve(lambda e: e.tensor_tensor(out=hb, in0=xt, in1=sh, op=ALU.add), reads=[B_x, B_m], writes=[B_hb])
            pt = bank_bf(pb)
            for k in range(8):
                P.pe(lambda e, k=k: e.transpose(out=pt[:, k * 128:(k + 1) * 128], in_=hb[:, k * 128:(k + 1) * 128],
                                                identity=ident_b), reads=[B_hb, B_cst], writes=[PSB[pb]])
            P.act(lambda e: e.copy(out=hT_dst, in_=pt.rearrange("p (a b) -> p a b", a=8)),
                  reads=[PSB[pb]], writes=[B_hT])

        def phase_attn():
            AR.off = persist_mark
            m0 = AR.off
            w_in = AR.alloc([8, 3 * D], BF16)
            B_win = Buf()
            for k in range(8 if DBG_W else 0):
                for c3 in range(3):
                    P.dma("pool", lambda e, k=k, c3=c3: e.dma_start(out=w_in[:, k, c3 * D:(c3 + 1) * D],
                                                                   in_=w_in_in[k * 128:(k + 1) * 128, c3 * D:(c3 + 1) * D]),
                          writes=[B_win])
            xt = [AR.alloc([D], F32) for _ in range(2)]
            hb = [AR.alloc([D], BF16) for _ in range(2)]
            hT = [AR.alloc([8, 512], BF16) for _ in range(2)]
            cs = [AR.alloc([2, 512], F32) for _ in range(2)]
            kraw = [AR.alloc([512], BF16) for _ in range(2)]
            t1 = [AR.alloc([512], F32) for _ in range(2)]
            t2 = [AR.alloc([512], F32) for _ in range(2)]
            kout = [AR.alloc([512], BF16) for _ in range(2)]
            vb = [AR.alloc([D], BF16) for _ in range(2)]
            B_xt, B_hb, B_hT, B_cs = [Buf(), Buf()], [Buf(), Buf()], [Buf(), Buf()], [Buf(), Buf()]
            B_kraw, B_t1, B_t2, B_kout, B_vb = ([Buf(), Buf()] for _ in range(5))
            ti = 0
            ci = 0
            for g in (list(range(17)) if DBG_G == 17 else list(range(DBG_G))):
                ctxg = (g == 16)
                ntile = 2 if ctxg else 4
                N = ntile * 128
                gb = g % 2
                if not ctxg:
                    P.dma("sp", lambda e, g=g, gb=gb: e.dma_start(out=cs[gb][:, 0, :], in_=cos_in[:, g * 512:(g + 1) * 512]),
                          writes=[B_cs[gb]])
                    P.dma("sp", lambda e, g=g, gb=gb: e.dma_start(out=cs[gb][:, 1, :], in_=sin_in[:, g * 512:(g + 1) * 512]),
                          writes=[B_cs[gb]])
                for t in range(ntile):
                    b2 = ti % 2
                    ti += 1
                    src = ctx_in[t * 128:(t + 1) * 128, :] if ctxg else x_in[g * 512 + t * 128:g * 512 + (t + 1) * 128, :]
                    P.dma("sp", lambda e, b2=b2, src=src: e.dma_start(out=xt[b2], in_=src), writes=[B_xt[b2]])
                    sc = modcc[:, 1, :] if ctxg else modbc[:, 1, :]
                    sh = modcc[:, 0, :] if ctxg else modbc[:, 0, :]
                    make_hT(xt[b2], B_xt[b2], sc, sh, B_modc if ctxg else B_mod, hb[b2], B_hb[b2], b2,
                            hT[gb][:, :, t * 128:(t + 1) * 128], B_hT[gb])
                jobs = [("k", j) for j in range(8)]
                if g < 8:
                    jobs += [("q", j) for j in range(8)]
                for (kind, j) in jobs[:DBG_J]:
                    c2 = ci % 2
                    ci += 1
                    pa, pbk = 2 + c2, 4 + c2
                    col0 = (D if kind == "k" else 0) + j * 128
                    for k in range(8):
                        P.pe(lambda e, k=k, pa=pa, col0=col0, gb=gb, N=N: e.matmul(
                            bank(pa)[:, :N], w_in[:, k, col0:col0 + 128], hT[gb][:, k, :N], start=(k == 0), stop=(k == 7)),
                            reads=[B_win, B_hT[gb]], writes=[PSB[pa]])
                    if DBG_K < 2:
                        continue
                    P.act(lambda e, c2=c2, pa=pa, N=N: e.copy(out=kraw[c2][:, :N], in_=bank(pa)[:, :N]),
                          reads=[PSB[pa]], writes=[B_kraw[c2]])
                    if DBG_K < 3:
                        continue
                    if ctxg or not DBG_R:
                        P.dma("sp", lambda e, c2=c2, j=j: e.dma_start(out=KT_d[j, :, NSEQ:NKEY], in_=kraw[c2][:, :NCTX]),
                              reads=[B_kraw[c2]], writes=[B_KT])
                        continue
                    P.pe(lambda e, c2=c2, pbk=pbk: e.matmul(bank(pbk), perm_b, kraw[c2], start=True, stop=True),
                         reads=[B_cst, B_kraw[c2]], writes=[PSB[pbk]])
                    if DBG_K < 4:
                        continue
                    P.dve(lambda e, c2=c2, pa=pa, gb=gb: e.tensor_tensor(out=t1[c2], in0=(kraw[c2] if DBG_X == 1 else bank(pa)), in1=(t2[c2] if DBG_X == 2 else cs[gb][:, 0, :]), op=ALU.mult),
                          reads=[PSB[pa], B_cs[gb], B_kraw[c2]], writes=[B_t1[c2]])
                    P.dve(lambda e, c2=c2, pbk=pbk, gb=gb: e.tensor_tensor(out=t2[c2], in0=bank(pbk), in1=cs[gb][:, 1, :], op=ALU.mult),
                          reads=[PSB[pbk], B_cs[gb]], writes=[B_t2[c2]])
                    if DBG_K < 5:
                        continue
                    P.pool(lambda e, c2=c2: e.tensor_tensor(out=kout[c2], in0=t1[c2], in1=t2[c2], op=ALU.add),
                           reads=[B_t1[c2], B_t2[c2]], writes=[B_kout[c2]])
                    if kind == "k":
                        P.dma("sp", lambda e, c2=c2, j=j, g=g: e.dma_start(out=KT_d[j, :, g * 512:(g + 1) * 512], in_=kout[c2]),
                              reads=[B_kout[c2]], writes=[B_KT])
                    else:
                        P.dma("sp", lambda e, c2=c2, j=j, g=g: e.dma_start(out=QT_d[j, :, g * 512:(g + 1) * 512], in_=kout[c2]),
                              reads=[B_kout[c2]], writes=[B_QT])
                for t in range(ntile if DBG_V else 0):
                    v2 = t % 2
                    for c in range(2):
                        for k in range(8):
                            P.pe(lambda e, k=k, c=c, t=t, gb=gb: e.matmul(
                                bank(6 + c), hT[gb][:, k, t * 128:(t + 1) * 128], w_in[:, k, 2 * D + c * 512:2 * D + (c + 1) * 512],
                                start=(k == 0), stop=(k == 7)), reads=[B_win, B_hT[gb]], writes=[PSB[6 + c]])
                    P.dve(lambda e, v2=v2: e.tensor_copy(out=vb[v2][:, 0:512], in_=bank(6)), reads=[PSB[6]], writes=[B_vb[v2]])
                    P.act(lambda e, v2=v2: e.copy(out=vb[v2][:, 512:1024], in_=bank(7)), reads=[PSB[7]], writes=[B_vb[v2]])
                    row0 = NSEQ + t * 128 if ctxg else g * 512 + t * 128
                    P.dma("act", lambda e, v2=v2, row0=row0: e.dma_start(out=V_d[row0:row0 + 128, :], in_=vb[v2]),
                          reads=[B_vb[v2]], writes=[B_V])
            P.barrier()
            if stop_after == "A1":
                AR.off = m0
                qb = AR.alloc([NTOK], BF16)
                qf = AR.alloc([NTOK], F32)
                Bq, Bf = Buf(), Buf()
                for j in range(8 if DBG_DUMP else 0):
                    P.dma("sp", lambda e, j=j: e.dma_start(out=qb, in_=QT_d[j]), reads=[B_QT], writes=[Bq])
                    P.dve(lambda e: e.tensor_copy(out=qf, in_=qb), reads=[Bq], writes=[Bf])
                    for c in range(4):
                        P.dma("sp", lambda e, j=j, c=c: e.dma_start(out=out_d[(j * 4 + c) * 128:(j * 4 + c + 1) * 128, :],
                                                                  in_=qf[:, c * 1024:(c + 1) * 1024]), reads=[Bf], writes=[B_OUT])
                return

            AR.off = m0
            onT = AR.alloc([8, NTOK], BF16)
            B_onT = Buf()
            m_on = AR.off
            KT = [AR.alloc([NKEY], BF16)] * 2
            Vh = [AR.alloc([66, 128], BF16)] * 2
            QT = [AR.alloc([NTOK], BF16)] * 2
            B_KTs, B_Vh, B_QTs = [Buf()] * 2, [Buf()] * 2, [Buf()] * 2
            E = [AR.alloc([512], BF16) for _ in range(4)]
            B_E = [Buf() for _ in range(4)]
            Es = [AR.alloc([512], F32) for _ in range(2)]
            B_Es = [Buf(), Buf()]
            r12 = [AR.alloc([512], F32) for _ in range(2)]
            B_r = [Buf(), Buf()]
            oT = AR.alloc([512], F32)
            o2 = AR.alloc([512], F32)
            sq = AR.alloc([512], F32)
            B_oT, B_o2, B_sq = Buf(), Buf(), Buf()
            lamt = AR.alloc([4 * 64], F32)
            lamw = AR.alloc([2 * 64], F32)
            lams = AR.alloc([4], F32)
            gcol = AR.alloc([1], F32)
            B_lam = Buf()
            P.dma("sp", lambda e: e.dma_start(out=lamt, in_=lam_in), writes=[B_lam])
            P.dma("sp", lambda e: e.dma_start(out=gcol, in_=subln_in), writes=[B_lam])
            P.dve(lambda e: e.tensor_tensor(out=lamw[:, 0:64], in0=lamt[:, 0:64], in1=lamt[:, 64:128], op=ALU.mult),
                  reads=[B_lam], writes=[B_lam])
            P.dve(lambda e: e.tensor_tensor(out=lamw[:, 64:128], in0=lamt[:, 128:192], in1=lamt[:, 192:256], op=ALU.mult),
                  reads=[B_lam], writes=[B_lam])
            P.dve(lambda e: e.tensor_reduce(out=lams[:, 0:1], in_=lamw[:, 0:64], axis=AX.X, op=ALU.add), reads=[B_lam], writes=[B_lam])
            P.dve(lambda e: e.tensor_reduce(out=lams[:, 1:2], in_=lamw[:, 64:128], axis=AX.X, op=ALU.add), reads=[B_lam], writes=[B_lam])
            P.act(lambda e: e.activation(out=lams[:, 0:2], in_=lams[:, 0:2], func=AF.Exp), reads=[B_lam], writes=[B_lam])
            P.dve(lambda e: e.tensor_tensor(out=lams[:, 2:3], in0=lams[:, 1:2], in1=lams[:, 0:1], op=ALU.subtract), reads=[B_lam], writes=[B_lam])
            P.dve(lambda e: e.tensor_scalar_add(out=lams[:, 3:4], in0=lams[:, 2:3], scalar1=-0.2), reads=[B_lam], writes=[B_lam])
            neg_lam = lams[:, 3:4]
            P.dve(lambda e: e.tensor_scalar_mul(out=gcol, in0=gcol, scalar1=0.8), reads=[B_lam], writes=[B_lam])

            for j in range(8):
                hb2 = j % 2
                P.dma("sp", lambda e, j=j, hb2=hb2: e.dma_start(out=KT[hb2], in_=KT_d[j]), reads=[B_KT], writes=[B_KTs[hb2]])
                P.dma("act", lambda e, j=j, hb2=hb2: e.dma_start(
                    out=Vh[hb2], in_=V_d[:, j * 128:(j + 1) * 128].rearrange("(t p) e -> p t e", p=128)),
                    reads=[B_V], writes=[B_Vh[hb2]])
                P.dma("sp", lambda e, j=j, hb2=hb2: e.dma_start(out=QT[hb2], in_=QT_d[j]), reads=[B_QT], writes=[B_QTs[hb2]])
                for g in range(8):
                    def qk(kt, m):
                        pbk = 2 * (kt % 2) + m
                        P.pe(lambda e, m=m, pbk=pbk, kt=kt, g=g, hb2=hb2: e.matmul(
                            bank(pbk), KT[hb2][m * 64:(m + 1) * 64, kt * 128:(kt + 1) * 128],
                            QT[hb2][m * 64:(m + 1) * 64, g * 512:(g + 1) * 512], start=True, stop=True),
                            reads=[B_KTs[hb2], B_QTs[hb2]], writes=[PSB[pbk]])
                    qk(0, 0)
                    qk(0, 1)
                    for kt in range(66):
                        if kt + 1 < 66:
                            qk(kt + 1, 0)
                            qk(kt + 1, 1)
                        for m in range(2):
                            pbk = 2 * (kt % 2) + m
                            ei = pbk
                            P.act(lambda e, pbk=pbk, ei=ei: e.activation(out=E[ei], in_=bank(pbk), func=AF.Exp, scale=0.125),
                                  reads=[PSB[pbk]], writes=[B_E[ei]])
                            P.pe(lambda e, m=m, ei=ei, kt=kt, hb2=hb2: e.matmul(
                                bank(4 + m), Vh[hb2][:, kt, :], E[ei], start=(kt == 0), stop=(kt == 65)),
                                reads=[B_Vh[hb2], B_E[ei]], writes=[PSB[4 + m]])
                            if m == 0:
                                a2 = kt % 2
                                if kt < 2:
                                    P.dve(lambda e, a2=a2, ei=ei: e.tensor_copy(out=Es[a2], in_=E[ei]),
                                          reads=[B_E[ei]], writes=[B_Es[a2]])
                                else:
                                    P.dve(lambda e, a2=a2, ei=ei: e.tensor_tensor(out=Es[a2], in0=Es[a2], in1=E[ei], op=ALU.add),
                                          reads=[B_E[ei], B_Es[a2]], writes=[B_Es[a2]])
                            else:
                                P.pe(lambda e, ei=ei, kt=kt: e.matmul(bank(6), ones_b, E[ei], start=(kt == 0), stop=(kt == 65)),
                                     reads=[B_cst, B_E[ei]], writes=[PSB[6]])
                    for a2 in range(2):
                        P.pe(lambda e, a2=a2: e.matmul(bank(7), ones_f, Es[a2], start=(a2 == 0), stop=(a2 == 1)),
                             reads=[B_cst, B_Es[a2]], writes=[PSB[7]])
                    P.dve(lambda e: e.reciprocal(out=r12[0], in_=bank(7)), reads=[PSB[7]], writes=[B_r[0]])
                    P.dve(lambda e: e.reciprocal(out=r12[1], in_=bank(6)), reads=[PSB[6]], writes=[B_r[1]])
                    P.dve(lambda e: e.tensor_tensor(out=oT, in0=bank(4), in1=r12[0], op=ALU.mult),
                          reads=[PSB[4], B_r[0]], writes=[B_oT])
                    P.dve(lambda e: e.tensor_tensor(out=o2, in0=bank(5), in1=r12[1], op=ALU.mult),
                          reads=[PSB[5], B_r[1]], writes=[B_o2])
                    P.dve(lambda e: e.scalar_tensor_tensor(out=oT, in0=o2, scalar=neg_lam, in1=oT, op0=ALU.mult, op1=ALU.add),
                          reads=[B_o2, B_oT, B_lam], writes=[B_oT])
                    P.pool(lambda e: e.tensor_tensor(out=sq, in0=oT, in1=oT, op=ALU.mult), reads=[B_oT], writes=[B_sq])
                    P.pe(lambda e: e.matmul(bank(7), onesd_f, sq, start=True, stop=True),
                         reads=[B_cst, B_sq], writes=[PSB[7]])
                    P.act(lambda e: e.activation(out=o2, in_=bank(7), func=AF.Sqrt, bias=eps_col, scale=1.0),
                          reads=[PSB[7], B_cst2], writes=[B_o2])
                    P.dve(lambda e: e.reciprocal(out=o2, in_=o2), reads=[B_o2], writes=[B_o2])
                    P.dve(lambda e, j=j, g=g: e.scalar_tensor_tensor(
                        out=onT[:, j, g * 512:(g + 1) * 512], in0=oT, scalar=gcol, in1=o2, op0=ALU.mult, op1=ALU.mult),
                        reads=[B_oT, B_o2, B_lam], writes=[B_onT])
            P.barrier()

            AR.off = m_on
            w_o = AR.alloc([8, D], BF16)
            B_wo = Buf()
            for k in range(8):
                P.dma("pool", lambda e, k=k: e.dma_start(out=w_o[:, k, :], in_=w_out_in[k * 128:(k + 1) * 128, :]),
                      writes=[B_wo])
            xr = [AR.alloc([D], F32) for _ in range(2)]
            zt = [AR.alloc([D], F32) for _ in range(2)]
            tmp = AR.alloc([D], F32)
            st4 = [AR.alloc([4], F32) for _ in range(2)]
            B_xr, B_zt, B_st = [Buf(), Buf()], [Buf(), Buf()], [Buf(), Buf()]
            B_tmp = Buf()
            for t in range(32):
                b2 = t % 2
                P.dma("sp", lambda e, t=t, b2=b2: e.dma_start(out=xr[b2], in_=x_in[t * 128:(t + 1) * 128, :]), writes=[B_xr[b2]])
                for c in range(2):
                    pbk = 2 * b2 + c
                    for j in range(8):
                        P.pe(lambda e, j=j, c=c, t=t, pbk=pbk: e.matmul(
                            bank(pbk), onT[:, j, t * 128:(t + 1) * 128], w_o[:, j, c * 512:(c + 1) * 512],
                            start=(j == 0), stop=(j == 7)), reads=[B_onT, B_wo], writes=[PSB[pbk]])
                    P.dve(lambda e, c=c, b2=b2, pbk=pbk: e.tensor_tensor(
                        out=zt[b2][:, c * 512:(c + 1) * 512], in0=bank(pbk), in1=modbc[:, 2, c * 512:(c + 1) * 512], op=ALU.mult),
                        reads=[PSB[pbk], B_mod], writes=[B_zt[b2]])
                P.dve(lambda e, b2=b2: e.scalar_tensor_tensor(out=zt[b2], in0=xr[b2], scalar=ALPHA, in1=zt[b2],
                                                              op0=ALU.mult, op1=ALU.add),
                      reads=[B_xr[b2], B_zt[b2]], writes=[B_zt[b2]])
                layer_norm_store(zt[b2], B_zt[b2], 0, X1_d[t * 128:(t + 1) * 128, :], B_X1, tmp, B_tmp, st4[b2], B_st[b2])
            P.barrier()

        def phase_sgu(Xin_d, B_in, Xout_d, B_out):
            AR.off = persist_mark
            m0 = AR.off
            w1 = AR.alloc([8, 4096], BF16)
            B_w1 = Buf()
            for k in range(8):
                for c4 in range(4):
                    P.dma("pool", lambda e, k=k, c4=c4: e.dma_start(out=w1[:, k, c4 * D:(c4 + 1) * D],
                                                                   in_=sgu_w_in_in[k * 128:(k + 1) * 128, c4 * D:(c4 + 1) * D]),
                          writes=[B_w1])
            ngb = AR.alloc([2, 2048], F32)
            binv = AR.alloc([2048], F32)
            binu = AR.alloc([16], F32)
            bs16 = AR.alloc([2048], F32)
            wsT = AR.alloc([8, 128], BF16)
            B_cc = Buf()
            P.dma("sp", lambda e: e.dma_start(out=ngb, in_=sgu_ngb_in.rearrange("a p d -> p a d")), writes=[B_cc])
            P.dma("sp", lambda e: e.dma_start(out=binv, in_=sgu_binv_in), writes=[B_cc])
            P.dma("sp", lambda e: e.dma_start(out=binu, in_=sgu_binu_in), writes=[B_cc])
            P.dma("sp", lambda e: e.dma_start(out=bs16, in_=sgu_bs16_in), writes=[B_cc])
            P.dma("pool", lambda e: e.dma_start(out=wsT, in_=sgu_wsT_in), writes=[B_cc])
            xt = [AR.alloc([D], F32) for _ in range(2)]
            hb = AR.alloc([D], BF16)
            hT = AR.alloc([8, 128], BF16)
            uT = AR.alloc([2048], BF16)
            vz = AR.alloc([2048], F32)
            vn = AR.alloc([2048], BF16)
            tT = [AR.alloc([2048], BF16) for _ in range(2)]
            st4 = AR.alloc([4], F32)
            B_xt, B_tT = [Buf(), Buf()], [Buf(), Buf()]
            B_hb, B_hT, B_uT, B_vz, B_vn, B_st = Buf(), Buf(), Buf(), Buf(), Buf(), Buf()
            mean, var, rstd, nmean = st4[:, 0:1], st4[:, 1:2], st4[:, 2:3], st4[:, 3:4]
            ps4a = psum[:, 0:4, :].rearrange("p a b -> p (a b)")
            ps4b = psum[:, 4:8, :].rearrange("p a b -> p (a b)")
            B_pa, B_pb = Buf(), Buf()
            for t in range(32):
                b2 = t % 2
                P.dma("sp", lambda e, t=t, b2=b2: e.dma_start(out=xt[b2], in_=Xin_d[t * 128:(t + 1) * 128, :]),
                      reads=[B_in], writes=[B_xt[b2]])
                P.dve(lambda e, b2=b2: e.tensor_tensor(out=xt[b2], in0=xt[b2], in1=modbc[:, 1, :], op=ALU.mult),
                      reads=[B_xt[b2], B_mod], writes=[B_xt[b2]])
                P.dve(lambda e, b2=b2: e.tensor_tensor(out=hb, in0=xt[b2], in1=modbc[:, 0, :], op=ALU.add),
                      reads=[B_xt[b2], B_mod], writes=[B_hb])
                pt = bank_bf(7)
                for k in range(8):
                    P.pe(lambda e, k=k: e.transpose(out=pt[:, k * 128:(k + 1) * 128], in_=hb[:, k * 128:(k + 1) * 128],
                                                    identity=ident_b), reads=[B_hb, B_cst], writes=[B_pb])
                P.act(lambda e: e.copy(out=hT, in_=pt.rearrange("p (a b) -> p a b", a=8)), reads=[B_pb], writes=[B_hT])
                for ch in range(16):
                    for k in range(8):
                        P.pe(lambda e, ch=ch, k=k: e.matmul(ps4a[:, ch * 128:(ch + 1) * 128], w1[:, k, ch * 128:(ch + 1) * 128],
                                                            hT[:, k, :], start=(k == 0), stop=(k == 7)),
                             reads=[B_w1, B_hT], writes=[B_pa])
                for ch in range(16):
                    P.act(lambda e, ch=ch: e.activation(out=uT[:, ch * 128:(ch + 1) * 128], in_=ps4a[:, ch * 128:(ch + 1) * 128],
                                                        func=AF.Gelu, bias=binu[:, ch:ch + 1], scale=1.0),
                          reads=[B_pa, B_cc], writes=[B_uT])
                for c in range(4):
                    for k in range(8):
                        P.pe(lambda e, c=c, k=k: e.matmul(ps4b[:, c * 512:(c + 1) * 512], hT[:, k, :],
                                                          w1[:, k, 2048 + c * 512:2048 + (c + 1) * 512], start=(k == 0), stop=(k == 7)),
                             reads=[B_w1, B_hT], writes=[B_pb])
                P.dve(lambda e: e.tensor_tensor(out=vz, in0=ps4b, in1=binv, op=ALU.add), reads=[B_pb, B_cc], writes=[B_vz])
                P.act(lambda e: e.activation(out=vz, in_=vz, func=AF.Gelu), reads=[B_vz], writes=[B_vz])
                P.dve(lambda e: e.tensor_reduce(out=mean, in_=vz, axis=AX.X, op=ALU.add), reads=[B_vz], writes=[B_st])
                P.dve(lambda e: e.tensor_scalar_mul(out=nmean, in0=mean, scalar1=-1.0 / 2048), reads=[B_st], writes=[B_st])
                P.act(lambda e: e.activation(out=vz, in_=vz, func=AF.Identity, bias=nmean, scale=1.0),
                      reads=[B_vz, B_st], writes=[B_vz])
                P.act(lambda e: e.activation(out=vn, in_=vz, func=AF.Square, accum_out=var), reads=[B_vz], writes=[B_vn, B_st])
                P.act(lambda e: e.activation(out=rstd, in_=var, func=AF.Sqrt, bias=eps_col, scale=1.0 / 2048),
                      reads=[B_st, B_cst2], writes=[B_st])
                P.dve(lambda e: e.reciprocal(out=rstd, in_=rstd), reads=[B_st], writes=[B_st])
                P.dve(lambda e: e.scalar_tensor_tensor(out=vz, in0=vz, scalar=rstd, in1=ngb[:, 0, :], op0=ALU.mult, op1=ALU.mult),
                      reads=[B_vz, B_st, B_cc], writes=[B_vz])
                P.dve(lambda e: e.tensor_tensor(out=vn, in0=vz, in1=ngb[:, 1, :], op=ALU.add), reads=[B_vz, B_cc, B_vn], writes=[B_vn])
                for ch in range(16):
                    P.pe(lambda e, ch=ch: e.matmul(ps4a[:, ch * 128:(ch + 1) * 128], vn[:, ch * 128:(ch + 1) * 128],
                                                   wsT[:, ch // 2, :], start=True, stop=True),
                         reads=[B_vn, B_cc], writes=[B_pa])
                P.dve(lambda e: e.tensor_tensor(out=vz, in0=ps4a, in1=bs16, op=ALU.add), reads=[B_pa, B_cc, B_vz], writes=[B_vz])
                P.dve(lambda e, b2=b2: e.tensor_tensor(out=tT[b2], in0=vz, in1=uT, op=ALU.mult), reads=[B_vz, B_uT], writes=[B_tT[b2]])
                P.dma("act", lambda e, t=t, b2=b2: e.dma_start(out=TT_d[t], in_=tT[b2]), reads=[B_tT[b2]], writes=[B_TT])
            P.barrier()
            AR.off = m0
            w2 = AR.alloc([16, D], BF16)
            B_w2 = Buf()
            for k in range(16):
                P.dma("pool", lambda e, k=k: e.dma_start(out=w2[:, k, :], in_=sgu_w_out_in[k * 128:(k + 1) * 128, :]),
                      writes=[B_w2])
            tl = [AR.alloc([2048], BF16) for _ in range(2)]
            xr = [AR.alloc([D], F32) for _ in range(2)]
            zt = [AR.alloc([D], F32) for _ in range(2)]
            tmp = AR.alloc([D], F32)
            s4 = [AR.alloc([4], F32) for _ in range(2)]
            B_tl, B_xr, B_zt, B_s4 = ([Buf(), Buf()] for _ in range(4))
            B_tmp = Buf()
            for t in range(32):
                b2 = t % 2
                P.dma("sp", lambda e, t=t, b2=b2: e.dma_start(out=xr[b2], in_=Xin_d[t * 128:(t + 1) * 128, :]),
                      reads=[B_in], writes=[B_xr[b2]])
                P.dma("act", lambda e, t=t, b2=b2: e.dma_start(out=tl[b2], in_=TT_d[t]), reads=[B_TT], writes=[B_tl[b2]])
                for c in range(2):
                    pbk = 2 * b2 + c
                    for ch in range(16):
                        P.pe(lambda e, ch=ch, c=c, b2=b2, pbk=pbk: e.matmul(
                            bank(pbk), tl[b2][:, ch * 128:(ch + 1) * 128], w2[:, ch, c * 512:(c + 1) * 512],
                            start=(ch == 0), stop=(ch == 15)), reads=[B_tl[b2], B_w2], writes=[PSB[pbk]])
                    P.dve(lambda e, c=c, b2=b2, pbk=pbk: e.tensor_tensor(
                        out=zt[b2][:, c * 512:(c + 1) * 512], in0=bank(pbk), in1=modbc[:, 2, c * 512:(c + 1) * 512], op=ALU.mult),
                        reads=[PSB[pbk], B_mod], writes=[B_zt[b2]])
                P.dve(lambda e, b2=b2: e.scalar_tensor_tensor(out=zt[b2], in0=xr[b2], scalar=ALPHA, in1=zt[b2],
                                                              op0=ALU.mult, op1=ALU.add),
                      reads=[B_xr[b2], B_zt[b2]], writes=[B_zt[b2]])
                layer_norm_store(zt[b2], B_zt[b2], 0, Xout_d[t * 128:(t + 1) * 128, :], B_out, tmp, B_tmp, s4[b2], B_s4[b2])
            P.barrier()

        def phase_moe(l, Xin_d, B_in, Xout_d, B_out):
            AR.off = persist_mark
            B_XS, B_YS, B_zf = Buf(), Buf(), Buf()
            dest_all = AR.alloc([32, 8], U32)
            w_all = AR.alloc([32, 8], F32)
            widx = AR.alloc([512], U32)
            B_dest, B_wall, B_widx = Buf(), Buf(), Buf()
            m_small = AR.off
            m_z = AR.off
            zt0 = AR.alloc([8192], BF16)
            AR.off = m_z
            wn_all = AR.alloc([32, 256], F32)
            B_z0 = Buf()
            P.pool(lambda e: e.memset(zt0, 0.0), writes=[B_z0])
            xsv = XS_d.rearrange("(n p r) d -> n p (r d)", p=128, r=8)
            for n in range(64):
                P.dma("act", lambda e, n=n: e.dma_start(out=xsv[n], in_=zt0), reads=[B_z0], writes=[B_zf])
            hball = AR.alloc([32, D], BF16)
            M_all = AR.alloc([32, 256], BF16)
            e8_all = AR.alloc([32, 8], F32)
            B_hball, B_Mall, B_wn, B_e8 = Buf(), Buf(), B_z0, Buf()
            w_r = AR.alloc([8, 256], BF16)
            rbias = AR.alloc([256], F32)
            B_wr = Buf()
            P.dma("pool", lambda e: e.dma_start(out=w_r, in_=router_w_in[l].rearrange("(k p) f -> p k f", p=128)), writes=[B_wr])
            P.dma("sp", lambda e: e.dma_start(out=rbias, in_=rbias_in[l]), writes=[B_wr])
            xt = [AR.alloc([D], F32)] * 2
            hT = AR.alloc([8, 128], BF16)
            sc = AR.alloc([256], F32)
            ch = AR.alloc([256], F32)
            mk = AR.alloc([256], F32)
            Mf = AR.alloc([256], F32)
            wt = AR.alloc([256], F32)
            m8 = AR.alloc([8, 8], F32)
            gsc = AR.alloc([8], F32)
            gs8 = AR.alloc([8], F32)
            gmk = AR.alloc([8], F32)
            t8 = AR.alloc([8], F32)
            i8 = AR.alloc([8], U32)
            ss = AR.alloc([2], F32)
            B_xt = [Buf()] * 2
            B_hT, B_r = Buf(), Buf()
            for t in range(32):
                b2 = t % 2
                P.dma("sp", lambda e, t=t, b2=b2: e.dma_start(out=xt[b2], in_=Xin_d[t * 128:(t + 1) * 128, :]),
                      reads=[B_in], writes=[B_xt[b2]])
                make_hT(xt[b2], B_xt[b2], modbc[:, 4, :], modbc[:, 3, :], B_mod, hball[:, t, :], B_hball, b2, hT, B_hT)
                for k in range(8):
                    P.pe(lambda e, k=k, b2=b2: e.matmul(bank(2 + b2)[:, 0:256], hT[:, k, :], w_r[:, k, :], start=(k == 0), stop=(k == 7)),
                         reads=[B_hT, B_wr], writes=[PSB[2 + b2]])
                P.act(lambda e, b2=b2: e.activation(out=sc, in_=bank(2 + b2)[:, 0:256], func=AF.Sigmoid), reads=[PSB[2 + b2]], writes=[B_r])
                P.dve(lambda e: e.tensor_tensor(out=ch, in0=sc, in1=rbias, op=ALU.add), reads=[B_r, B_wr], writes=[B_r])
                for g in range(8):
                    P.dve(lambda e, g=g: e.max(out=m8[:, g, :], in_=ch[:, g * 32:(g + 1) * 32]), reads=[B_r], writes=[B_r])
                P.dve(lambda e: e.tensor_tensor(out=gsc, in0=m8[:, :, 0], in1=m8[:, :, 1], op=ALU.add), reads=[B_r], writes=[B_r])
                P.dve(lambda e: e.max(out=gs8, in_=gsc), reads=[B_r], writes=[B_r])
                P.dve(lambda e: e.tensor_scalar(out=gmk, in0=gsc, scalar1=gs8[:, 3:4], scalar2=None, op0=ALU.is_ge), reads=[B_r], writes=[B_r])
                P.dve(lambda e: e.tensor_scalar(out=gmk, in0=gmk, scalar1=1e30, scalar2=-1e30, op0=ALU.mult, op1=ALU.add), reads=[B_r], writes=[B_r])
                P.dve(lambda e: e.tensor_tensor(out=mk.rearrange("p (g c) -> p g c", g=8), in0=ch.rearrange("p (g c) -> p g c", g=8),
                                                in1=gmk.unsqueeze(2).to_broadcast([128, 8, 32]), op=ALU.add), reads=[B_r], writes=[B_r])
                P.dve(lambda e: e.max(out=t8, in_=mk), reads=[B_r], writes=[B_r])
                P.dve(lambda e: e.tensor_scalar(out=Mf, in0=mk, scalar1=t8[:, 7:8], scalar2=None, op0=ALU.is_ge), reads=[B_r], writes=[B_r])
                P.dve(lambda e, t=t: e.tensor_copy(out=M_all[:, t, :], in_=Mf), reads=[B_r], writes=[B_Mall])
                P.dve(lambda e: e.scalar_tensor_tensor(out=wt, in0=sc, scalar=1.0, in1=Mf, op0=ALU.mult, op1=ALU.mult, accum_out=ss[:, 0:1]),
                      reads=[B_r], writes=[B_r])
                P.dve(lambda e: e.reciprocal(out=ss[:, 1:2], in_=ss[:, 0:1]), reads=[B_r], writes=[B_r])
                P.dve(lambda e, t=t: e.tensor_scalar(out=wn_all[:, t, :], in0=wt, scalar1=ss[:, 1:2], scalar2=2.5, op0=ALU.mult, op1=ALU.mult),
                      reads=[B_r], writes=[B_wn])
                P.dve(lambda e: e.max_index(out=i8, in_max=t8, in_values=mk), reads=[B_r], writes=[B_r])
                P.dve(lambda e, t=t: e.tensor_scalar_add(out=e8_all[:, t, :], in0=i8, scalar1=0.0), reads=[B_r], writes=[B_e8])
                P.pe(lambda e, t=t: e.matmul(bank(7)[:, 0:256], ones_b, M_all[:, t, :], start=(t == 0), stop=(t == 31)),
                     reads=[B_Mall, B_cst], writes=[PSB[7]])
            if DBG_M < 2:
                P.barrier()
                return
            cnt = AR.alloc([256], F32)
            ci = AR.alloc([256], I32)
            pad = AR.alloc([256], F32)
            cs_a = AR.alloc([256], F32)
            cs_b = AR.alloc([256], F32)
            pstart = AR.alloc([256], F32)
            pcol = AR.alloc([2], F32)
            junk = AR.alloc([256], F32)
            Ab = [AR.alloc([512], BF16) for _ in range(2)]
            bef = AR.alloc([512], F32)
            B_l = Buf()
            P.dve(lambda e: e.tensor_scalar_add(out=ci, in0=bank(7)[:, 0:256], scalar1=127.0), reads=[PSB[7]], writes=[B_l])
            P.dve(lambda e: e.tensor_single_scalar(out=ci, in_=ci, scalar=7, op=ALU.arith_shift_right), reads=[B_l], writes=[B_l])
            P.dve(lambda e: e.tensor_single_scalar(out=ci, in_=ci, scalar=7, op=ALU.logical_shift_left), reads=[B_l], writes=[B_l])
            P.dve(lambda e: e.tensor_scalar_add(out=pad, in0=ci, scalar1=0.0), reads=[B_l], writes=[B_l])
            P.dve(lambda e: e.tensor_copy(out=cs_a, in_=pad), reads=[B_l], writes=[B_l])
            cur, nxt = cs_a, cs_b
            sft = 1
            while sft < 256:
                P.dve(lambda e, cur=cur, nxt=nxt, sft=sft: e.tensor_copy(out=nxt[:, 0:sft], in_=cur[:, 0:sft]), reads=[B_l], writes=[B_l])
                P.dve(lambda e, cur=cur, nxt=nxt, sft=sft: e.tensor_tensor(out=nxt[:, sft:256], in0=cur[:, sft:256], in1=cur[:, 0:256 - sft], op=ALU.add),
                      reads=[B_l], writes=[B_l])
                cur, nxt = nxt, cur
                sft *= 2
            pends = cur
            P.dve(lambda e: e.tensor_tensor(out=pstart, in0=pends, in1=pad, op=ALU.subtract), reads=[B_l], writes=[B_l])
            for c in range(2):
                P.dve(lambda e, c=c: e.scalar_tensor_tensor(out=junk[:, 0:128], in0=pends[:, c * 128:(c + 1) * 128], scalar=1.0, in1=ident_f,
                                                            op0=ALU.mult, op1=ALU.mult, accum_out=pcol[:, c:c + 1]),
                      reads=[B_l, B_cst], writes=[B_l])
                P.dve(lambda e, c=c: e.tensor_scalar(out=Ab[c], in0=iota_blk, scalar1=pcol[:, c:c + 1], scalar2=None, op0=ALU.is_ge),
                      reads=[B_l, B_cst], writes=[B_l])
                P.pe(lambda e, c=c: e.matmul(bank(6), ones_b, Ab[c], start=(c == 0), stop=(c == 1)), reads=[B_l, B_cst], writes=[PSB[6]])
            P.dve(lambda e: e.tensor_scalar_min(out=bef, in0=bank(6), scalar1=255.0), reads=[PSB[6]], writes=[B_l])
            P.dve(lambda e: e.tensor_scalar(out=bef, in0=bef, scalar1=128.0, scalar2=iota_p, op0=ALU.mult, op1=ALU.add),
                  reads=[B_l, B_cst], writes=[B_l])
            P.dve(lambda e: e.tensor_scalar_add(out=widx, in0=bef, scalar1=float(l * NE * 128)), reads=[B_l], writes=[B_widx])
            if DBG_M < 3:
                P.barrier()
                return
            dm = AR.alloc([256], F32)
            d8 = AR.alloc([8], F32)
            B_dm = Buf()
            for t in range(32):
                b2 = t % 2
                P.pe(lambda e, t=t, b2=b2: e.matmul(bank(b2)[:, 0:256], ustr_b, M_all[:, t, :], start=True, stop=(t == 0)),
                     reads=[B_Mall, B_cst], writes=[PSB[b2]])
                for j in range(t):
                    P.pe(lambda e, j=j, t=t, b2=b2: e.matmul(bank(b2)[:, 0:256], ones_b, M_all[:, j, :], start=False, stop=(j == t - 1)),
                         reads=[B_Mall, B_cst], writes=[PSB[b2]])
                P.dve(lambda e, b2=b2: e.tensor_tensor(out=dm, in0=bank(b2)[:, 0:256], in1=pstart, op=ALU.add), reads=[PSB[b2], B_l], writes=[B_dm])
                for j in range(8):
                    P.dve(lambda e, t=t, j=j: e.scalar_tensor_tensor(out=junk, in0=iota_e, scalar=e8_all[:, t, j:j + 1], in1=dm,
                                                                     op0=ALU.is_equal, op1=ALU.mult, accum_out=d8[:, j:j + 1]),
                          reads=[B_e8, B_dm, B_cst], writes=[B_dm])
                    P.dve(lambda e, t=t, j=j: e.scalar_tensor_tensor(out=junk, in0=iota_e, scalar=e8_all[:, t, j:j + 1], in1=wn_all[:, t, :],
                                                                     op0=ALU.is_equal, op1=ALU.mult, accum_out=w_all[:, t, j:j + 1]),
                          reads=[B_e8, B_wn, B_cst], writes=[B_wall])
                P.dve(lambda e, t=t: e.tensor_scalar_add(out=dest_all[:, t, :], in0=d8, scalar1=0.0), reads=[B_dm], writes=[B_dest])
                if DBG_M == 3:
                    dbg = AR.alloc([24], F32) if t == 0 else dbg
                    B_dbg = Buf() if t == 0 else B_dbg
                    P.dve(lambda e, dbg=dbg: e.tensor_copy(out=dbg[:, 0:8], in_=d8), reads=[B_dm], writes=[B_dbg])
                    P.dve(lambda e, t=t, dbg=dbg: e.tensor_copy(out=dbg[:, 8:16], in_=w_all[:, t, :]), reads=[B_wall], writes=[B_dbg])
                    P.dve(lambda e, t=t, dbg=dbg: e.tensor_copy(out=dbg[:, 16:24], in_=e8_all[:, t, :]), reads=[B_e8], writes=[B_dbg])
                    P.dma("sp", lambda e, t=t, dbg=dbg: e.dma_start(out=out_d[t * 128:(t + 1) * 128, 0:24], in_=dbg), reads=[B_dbg], writes=[B_OUT])
                for j in range(8 if DBG_M >= 4 else 0):
                    P.dma("pool", lambda e, t=t, j=j: e.indirect_dma_start(
                        out=XS_d, out_offset=bass.IndirectOffsetOnAxis(ap=dest_all[:, t, j:j + 1], axis=0),
                        in_=hball[:, t, :], in_offset=None), reads=[B_dest, B_hball, B_zf], writes=[B_XS])
            P.barrier()
            if DBG_M < 5:
                return
            AR.off = m_small
            wg = [AR.alloc([2048], BF16) for _ in range(3)]
            wu = [AR.alloc([2048], BF16) for _ in range(3)]
            wd = [AR.alloc([2048], BF16) for _ in range(3)]
            xb = [AR.alloc([D], BF16) for _ in range(2)]
            xT = [AR.alloc([8, 128], BF16) for _ in range(2)]
            g32 = AR.alloc([256], F32)
            Gb = [AR.alloc([256], BF16) for _ in range(2)]
            GT = AR.alloc([2, 128], BF16)
            yb = [AR.alloc([D], BF16) for _ in range(2)]
            B_wgu, B_wd = [Buf() for _ in range(3)], [Buf() for _ in range(3)]
            B_xb, B_xT, B_yb, B_G = ([Buf(), Buf()] for _ in range(4))
            B_g32, B_GT = Buf(), Buf()

            def stage1(b):
                b2, b3 = b % 2, b % 3
                off = bass.IndirectOffsetOnAxis(ap=widx[:, b:b + 1], axis=0)
                P.dma("pool", lambda e: e.indirect_dma_start(out=wg[b3], out_offset=None, in_=exp_g_in, in_offset=off),
                      reads=[B_widx], writes=[B_wgu[b3]])
                P.dma("pool", lambda e: e.indirect_dma_start(out=wu[b3], out_offset=None, in_=exp_u_in, in_offset=off),
                      reads=[B_widx], writes=[B_wgu[b3]])
                P.dma("pool", lambda e: e.indirect_dma_start(out=wd[b3], out_offset=None, in_=exp_d_in, in_offset=off),
                      reads=[B_widx], writes=[B_wd[b3]])
                P.dma("sp", lambda e: e.dma_start(out=xb[b2], in_=XS_d[b * 128:(b + 1) * 128, :]), reads=[B_XS], writes=[B_xb[b2]])
                pt = bank_bf(b2)
                xv = xb[b2].rearrange("s (p k) -> s k p", k=8)
                for k in range(8):
                    P.pe(lambda e, k=k: e.transpose(out=pt[:, k * 128:(k + 1) * 128], in_=xv[:, k, :], identity=ident_b),
                         reads=[B_xb[b2], B_cst], writes=[PSB[b2]])
                P.act(lambda e: e.copy(out=xT[b2], in_=pt.rearrange("p (a b) -> p a b", a=8)), reads=[PSB[b2]], writes=[B_xT[b2]])
                for (wsrc, c0) in ((wg, 0), (wu, 256)):
                    for k in range(8):
                        P.pe(lambda e, k=k, wsrc=wsrc, c0=c0: e.matmul(bank(2 + b2)[:, c0:c0 + 256], xT[b2][:, k, :],
                                                                       wsrc[b3][:, k * 256:(k + 1) * 256], start=(k == 0), stop=(k == 7)),
                             reads=[B_xT[b2], B_wgu[b3]], writes=[PSB[2 + b2]])
                P.act(lambda e: e.activation(out=g32, in_=bank(2 + b2)[:, 0:256], func=AF.Silu), reads=[PSB[2 + b2]], writes=[B_g32])
                P.dve(lambda e: e.tensor_tensor(out=Gb[b2], in0=bank(2 + b2)[:, 256:512], in1=g32, op=ALU.mult),
                      reads=[PSB[2 + b2], B_g32], writes=[B_G[b2]])

            def stage2(b):
                b2, b3 = b % 2, b % 3
                gv = Gb[b2].rearrange("s (p j) -> s j p", j=2)
                pg = bank_bf(4 + b2)
                for j in range(2):
                    P.pe(lambda e, j=j: e.transpose(out=pg[:, j * 128:(j + 1) * 128], in_=gv[:, j, :], identity=ident_b),
                         reads=[B_G[b2], B_cst], writes=[PSB[4 + b2]])
                P.dve(lambda e: e.tensor_copy(out=GT, in_=pg[:, 0:256].rearrange("p (a b) -> p a b", a=2)), reads=[PSB[4 + b2]], writes=[B_GT])
                for c in range(2):
                    for j in range(2):
                        P.pe(lambda e, c=c, j=j: e.matmul(bank(6 + c), GT[:, j, :], wd[b3][:, j * D + c * 512:j * D + (c + 1) * 512],
                                                          start=(j == 0), stop=(j == 1)), reads=[B_GT, B_wd[b3]], writes=[PSB[6 + c]])
                P.dve(lambda e: e.tensor_copy(out=yb[b2][:, 0:512], in_=bank(6)), reads=[PSB[6]], writes=[B_yb[b2]])
                P.act(lambda e: e.copy(out=yb[b2][:, 512:1024], in_=bank(7)), reads=[PSB[7]], writes=[B_yb[b2]])
                P.dma("act", lambda e: e.dma_start(out=YS_d[b * 128:(b + 1) * 128, :], in_=yb[b2]), reads=[B_yb[b2]], writes=[B_YS])

            stage1(0)
            for b in range(NBLK):
                if b + 1 < NBLK:
                    stage1(b + 1)
                stage2(b)
            P.barrier()
            if DBG_M < 6:
                return
            AR.off = m_small
            sgu_w = AR.alloc([8, 512], BF16)
            sdw = AR.alloc([2, D], BF16)
            B_sw = Buf()
            P.dma("pool", lambda e: e.dma_start(out=sgu_w[:, :, 0:256], in_=sh_g_in[l].rearrange("(k p) f -> p k f", p=128)), writes=[B_sw])
            P.dma("pool", lambda e: e.dma_start(out=sgu_w[:, :, 256:512], in_=sh_u_in[l].rearrange("(k p) f -> p k f", p=128)), writes=[B_sw])
            P.dma("pool", lambda e: e.dma_start(out=sdw, in_=sh_d_in[l].rearrange("(j p) d -> p j d", p=128)), writes=[B_sw])
            g32s = AR.alloc([256], F32)
            Gbs = AR.alloc([256], BF16)
            GTs = AR.alloc([2, 128], BF16)
            B_g32s, B_GTs, B_Gs = Buf(), Buf(), Buf()
            yg = [AR.alloc([D], BF16) for _ in range(8)]
            B_yg = [Buf() for _ in range(8)]
            acc = AR.alloc([D], F32)
            xt2 = AR.alloc([D], F32)
            xr = AR.alloc([D], F32)
            hb2 = AR.alloc([D], BF16)
            hT2 = AR.alloc([8, 128], BF16)
            zt = AR.alloc([D], F32)
            tmp = AR.alloc([D], F32)
            s4 = AR.alloc([4], F32)
            B_acc, B_xt2, B_xr, B_hb2, B_hT2, B_zt, B_tmp, B_s4 = (Buf() for _ in range(8))
            for t in range(32):
                for j in range(8):
                    P.dma("pool", lambda e, t=t, j=j: e.indirect_dma_start(
                        out=yg[j], out_offset=None, in_=YS_d, in_offset=bass.IndirectOffsetOnAxis(ap=dest_all[:, t, j:j + 1], axis=0)),
                        reads=[B_YS, B_dest], writes=[B_yg[j]])
                P.dve(lambda e, t=t: e.tensor_scalar_mul(out=acc, in0=yg[0], scalar1=w_all[:, t, 0:1]), reads=[B_yg[0], B_wall], writes=[B_acc])
                for j in range(1, 8):
                    P.dve(lambda e, t=t, j=j: e.scalar_tensor_tensor(out=acc, in0=yg[j], scalar=w_all[:, t, j:j + 1], in1=acc,
                                                                     op0=ALU.mult, op1=ALU.add), reads=[B_yg[j], B_wall, B_acc], writes=[B_acc])
                P.dma("sp", lambda e, t=t: e.dma_start(out=xt2, in_=Xin_d[t * 128:(t + 1) * 128, :]), reads=[B_in], writes=[B_xt2])
                P.dma("sp", lambda e, t=t: e.dma_start(out=xr, in_=Xin_d[t * 128:(t + 1) * 128, :]), reads=[B_in], writes=[B_xr])
                make_hT(xt2, B_xt2, modbc[:, 4, :], modbc[:, 3, :], B_mod, hb2, B_hb2, 0, hT2, B_hT2)
                for k in range(8):
                    P.pe(lambda e, k=k: e.matmul(bank(2), hT2[:, k, :], sgu_w[:, k, :], start=(k == 0), stop=(k == 7)),
                         reads=[B_hT2, B_sw], writes=[PSB[2]])
                P.act(lambda e: e.activation(out=g32s, in_=bank(2)[:, 0:256], func=AF.Silu), reads=[PSB[2]], writes=[B_g32s])
                P.dve(lambda e: e.tensor_tensor(out=Gbs, in0=bank(2)[:, 256:512], in1=g32s, op=ALU.mult), reads=[PSB[2], B_g32s], writes=[B_Gs])
                pg = bank_bf(4)
                for j in range(2):
                    P.pe(lambda e, j=j, pg=pg: e.transpose(out=pg[:, j * 128:(j + 1) * 128], in_=Gbs[:, j * 128:(j + 1) * 128], identity=ident_b),
                         reads=[B_Gs, B_cst], writes=[PSB[4]])
                P.dve(lambda e, pg=pg: e.tensor_copy(out=GTs, in_=pg[:, 0:256].rearrange("p (a b) -> p a b", a=2)), reads=[PSB[4]], writes=[B_GTs])
                for c in range(2):
                    for j in range(2):
                        P.pe(lambda e, c=c, j=j: e.matmul(bank(6 + c), GTs[:, j, :], sdw[:, j, c * 512:(c + 1) * 512], start=(j == 0), stop=(j == 1)),
                             reads=[B_GTs, B_sw], writes=[PSB[6 + c]])
                    P.dve(lambda e, c=c: e.tensor_tensor(out=zt[:, c * 512:(c + 1) * 512], in0=bank(6 + c), in1=acc[:, c * 512:(c + 1) * 512], op=ALU.add),
                          reads=[PSB[6 + c], B_acc], writes=[B_zt])
                P.dve(lambda e: e.tensor_tensor(out=zt, in0=zt, in1=modbc[:, 5, :], op=ALU.mult), reads=[B_zt, B_mod], writes=[B_zt])
                P.dve(lambda e: e.scalar_tensor_tensor(out=zt, in0=xr, scalar=ALPHA, in1=zt, op0=ALU.mult, op1=ALU.add),
                      reads=[B_xr, B_zt], writes=[B_zt])
                layer_norm_store(zt, B_zt, 1, Xout_d[t * 128:(t + 1) * 128, :], B_out, tmp, B_tmp, s4, B_s4)
            P.barrier()

        if stop_after == "Btest":
            phase_mod(0, False)
            phase_moe(0, x_in, Buf(), out_d, B_OUT)
            P.emit(nc, st)
            return nc, P
        if stop_after == "Ctest":
            phase_mod(1, False)
            phase_sgu(x_in, Buf(), out_d, B_OUT)
            P.emit(nc, st)
            return nc, P
        phase_mod(0, True)
        if stop_after == "M":
            for ch in range(6):
                P.dma("sp", lambda e, ch=ch: e.dma_start(out=out_d[ch * 128:(ch + 1) * 128, :], in_=modbc[:, ch, :]),
                      reads=[B_mod], writes=[B_OUT])
            for ch in range(2):
                P.dma("sp", lambda e, ch=ch: e.dma_start(out=out_d[(6 + ch) * 128:(7 + ch) * 128, :], in_=modcc[:, ch, :]),
                      reads=[B_modc], writes=[B_OUT])
            P.emit(nc, st)
            return nc, P
        phase_attn()
        if stop_after == "D":
            phase_moe(0, X1_d, B_X1, X2_d, B_X2)
            phase_mod(1, False)
            phase_sgu(X2_d, B_X2, X3_d, B_X3)
            phase_moe(1, X3_d, B_X3, out_d, B_OUT)
            P.emit(nc, st)
            return nc, P
        if stop_after == "A1":
            P.emit(nc, st)
            return nc, P
        if stop_after == "A":
            AR.off = persist_mark
            cp = [AR.alloc([D], F32) for _ in range(2)]
            B_cp = [Buf(), Buf()]
            for t in range(32):
                b2 = t % 2
                P.dma("sp", lambda e, t=t, b2=b2: e.dma_start(out=cp[b2], in_=X1_d[t * 128:(t + 1) * 128, :]),
                      reads=[B_X1], writes=[B_cp[b2]])
                P.dma("sp", lambda e, t=t, b2=b2: e.dma_start(out=out_d[t * 128:(t + 1) * 128, :], in_=cp[b2]),
                      reads=[B_cp[b2]], writes=[B_OUT])
        P.emit(nc, st)
    return nc, P


def _consts():
    cst = np.zeros((128, 5, 128), np.float32)
    cst[:, 0, :] = np.eye(128, dtype=np.float32)
    cst[:, 1, :] = 1.0
    cst[:, 2, :] = np.triu(np.ones((128, 128), np.float32), 1)
    perm = np.zeros((128, 128), np.float32)
    for dst in range(128):
        i = dst % 32
        src = dst + 16 if i < 16 else dst - 16
        perm[src, dst] = 1.0
    cst[:, 3, :] = perm
    cst[:, 4, :] = 1.0 / 128.0
    iota = np.zeros((128, 256 + 512 + 1), np.float32)
    iota[:, 0:256] = np.arange(256, dtype=np.float32)[None, :]
    iota[:, 256:768] = (np.arange(512, dtype=np.float32) * 128.0)[None, :]
    iota[:, 768] = np.arange(128, dtype=np.float32)
    return cst, iota


def _rope_tables():
    n = np.arange(NSEQ)
    row = (n // 64).astype(np.float32)
    col = (n % 64).astype(np.float32)
    inv = (10000.0 ** (-np.arange(0, 32, 2, dtype=np.float32) / np.float32(32))).astype(np.float32)
    cosT = np.zeros((128, NSEQ), np.float32)
    sinT = np.zeros((128, NSEQ), np.float32)
    for p in range(128):
        i = p % 64
        pos = row if i < 32 else col
        jj = i % 32
        ang = (pos * inv[jj % 16]).astype(np.float32)
        cosT[p] = np.cos(ang)
        sinT[p] = np.sin(ang) * (-1.0 if jj < 16 else 1.0)
    return cosT, sinT


def _bc(v):
    return np.ascontiguousarray(np.broadcast_to(np.asarray(v, np.float32).reshape(1, -1), (128, v.size)))


_CACHE = {}


def make_in_maps(inputs, cores, stop_after):
    cst, iota = _consts()
    cosT, sinT = _rope_tables()
    f = lambda k: np.asarray(inputs[k], np.float32)
    x, c, ctx, c_ctx = f("x"), f("c"), f("ctx"), f("c_ctx")
    w_mod, b_mod, ln_g, ln_b = f("w_mod"), f("b_mod"), f("ln_g"), f("ln_b")
    b_mod_bc = np.ascontiguousarray(np.broadcast_to(b_mod[:, None, :], (2, 128, 6 * D)))
    ln_bc = np.zeros((2, 2, 2, 128, D), np.float32)
    for l in range(2):
        for a in range(2):
            ln_bc[l, a, 0] = ln_g[l, a][None, :]
            ln_bc[l, a, 1] = ln_b[l, a][None, :]
    maps = []
    for core in cores:
        b, half = core // 2, core % 2
        order = np.concatenate([np.arange(half * NTOK, (half + 1) * NTOK), np.arange((1 - half) * NTOK, (2 - half) * NTOK)])
        m = {
            "x": np.ascontiguousarray(x[b][order]),
            "ctx": np.ascontiguousarray(ctx[b]),
            "c_bc": _bc(c[b]), "cc_bc": _bc(c_ctx),
            "w_mod": w_mod, "b_mod_bc": b_mod_bc, "ln_bc": ln_bc,
            "attn_w_in": f("attn_w_in")[0], "attn_w_out": f("attn_w_out")[0],
            "lam_bc": _bc(f("attn_lambda")[0].reshape(-1)),
            "subln_col": np.ascontiguousarray(f("attn_subln_g")[0].reshape(128, 1)),
            "cosT": np.ascontiguousarray(cosT[:, order]), "sinT": np.ascontiguousarray(sinT[:, order]),
            "cst": cst, "iota": iota,
            "router_w": f("router_w"), "rbias_bc": np.ascontiguousarray(np.broadcast_to(f("router_bias")[:, None, :], (2, 128, NE))),
            "exp_w_gate": f("exp_w_gate").reshape(2 * NE * 128, 2048), "exp_w_up": f("exp_w_up").reshape(2 * NE * 128, 2048),
            "exp_w_down": f("exp_w_down").reshape(2 * NE * 128, 2048),
            "sh_w_gate": f("sh_w_gate"), "sh_w_up": f("sh_w_up"), "sh_w_down": f("sh_w_down"),
            "sgu_w_in": f("sgu_w_in")[0], "sgu_w_out": f("sgu_w_out")[0],
            "sgu_ngb": np.stack([_bc(f("sgu_norm_g")[0]), _bc(f("sgu_norm_b")[0])]),
            "sgu_binv": _bc(f("sgu_b_in")[0][2048:]),
            "sgu_binu": np.ascontiguousarray(f("sgu_b_in")[0][:2048].reshape(16, 128).T),
            "sgu_bs16": _bc(np.repeat(f("sgu_b_s")[0], 2, axis=0).reshape(-1)),
            "sgu_wsT": np.ascontiguousarray(f("sgu_w_s")[0].transpose(2, 0, 1)),
        }
        maps.append(m)
    return maps


def kernel(**inputs):
    stop_after = "D"
    if "nc" not in _CACHE:
        _CACHE["nc"] = build(stop_after)[0]
    nc = _CACHE["nc"]
    cores = list(range(8))
    maps = make_in_maps(inputs, cores, stop_after)
    res = run_bass_kernel_spmd(nc, maps, core_ids=cores)
    out = np.zeros((4, NSEQ, D), np.float32)
    for core in cores:
        b, half = core // 2, core % 2
        out[b, half * NTOK:(half + 1) * NTOK] = res.results[core]["out"]
    return out
```
